# Optimizing a Trainium2 kernel written in Bass

```python
import jax, jax.numpy as jnp
from jax import lax
import numpy as np

D_MODEL = 1024
BATCH = 4
SEQ = 4096
DEPTH = 2

CHUNK = 64
N_EVEN = (DEPTH + 1) // 2
N_ODD = DEPTH // 2
EPS = 1e-6
ROPE_THETA = 10000.0

GM_GROUPS = 4
GM_GROUP_DIM = 128
GM_WIDTH = GM_GROUPS * GM_GROUP_DIM
GM_BLOCK = 128
MLA_HEADS = 8
MLA_Q_RANK = 384
MLA_KV_RANK = 256
MLA_NOPE = 64
MLA_ROPE = 32
MLA_V = 64
MLA_QK = MLA_NOPE + MLA_ROPE
ATTN_BLOCK = 128
EVEN_IN = 2 * GM_WIDTH + MLA_Q_RANK + MLA_KV_RANK + MLA_ROPE
EVEN_MIX = GM_WIDTH + MLA_HEADS * MLA_V
RET_HEADS = 4
RET_QK = 256
RET_V = 512
ODD_IN = 2 * RET_HEADS * RET_QK + 2 * RET_HEADS * RET_V
ODD_MIX = RET_HEADS * RET_V
D_FF = -(-8 * D_MODEL // (3 * 256)) * 256

kernel_name = "hybrid_gmlp_mla_retention_trunk"


def rmsnorm(x, g):
    xf = x.astype(jnp.float32)
    y = xf * lax.rsqrt(jnp.mean(xf * xf, axis=-1, keepdims=True) + EPS)
    return (y * g.astype(jnp.float32)).astype(x.dtype)


def rope(x, pos):
    half = x.shape[-1] // 2
    inv = ROPE_THETA ** (-jnp.arange(half, dtype=jnp.float32) / half)
    ang = pos.astype(jnp.float32)[:, None] * inv[None, :]
    cos = jnp.cos(ang)[None, :, None, :]
    sin = jnp.sin(ang)[None, :, None, :]
    xf = x.astype(jnp.float32)
    x1, x2 = xf[..., :half], xf[..., half:]
    return jnp.concatenate([x1 * cos - x2 * sin, x2 * cos + x1 * sin], axis=-1).astype(x.dtype)


def spatial_gating(u, v, v_norm, w_s, b_s):
    B, S, _ = u.shape
    n = S // GM_BLOCK
    v = rmsnorm(v.reshape(B, S, GM_GROUPS, GM_GROUP_DIM), v_norm)
    cid = jnp.arange(GM_BLOCK) // CHUNK
    w = jnp.where((cid[None, :] <= cid[:, None])[None], w_s, 0)
    vb = v.reshape(B, n, GM_BLOCK, GM_GROUPS, GM_GROUP_DIM)
    s = jnp.einsum('gts,bnsgc->bntgc', w, vb) + b_s.T[:, :, None]
    ub = u.reshape(B, n, GM_BLOCK, GM_GROUPS, GM_GROUP_DIM)
    return (ub * s).reshape(B, S, GM_WIDTH)


def block_causal_attention(q, k, v):
    S = q.shape[1]
    scale = MLA_QK ** -0.5
    chunk_id = jnp.arange(S) // CHUNK
    outs = []
    for start in range(0, S, ATTN_BLOCK):
        end = start + ATTN_BLOCK
        s = jnp.einsum('bqhd,bkhd->bhqk', q[:, start:end], k[:, :end]).astype(jnp.float32) * scale
        mask = chunk_id[None, :end] <= chunk_id[start:end, None]
        p = jax.nn.softmax(jnp.where(mask, s, -jnp.inf), axis=-1).astype(v.dtype)
        outs.append(jnp.einsum('bhqk,bkhd->bqhd', p, v[:, :end]))
    return jnp.concatenate(outs, axis=1)


def mla(c_q, c_kv, k_pe, q_a_norm, w_uq, kv_a_norm, w_ukv, q_norm, k_norm, pos):
    B, S, _ = c_q.shape
    q = (rmsnorm(c_q, q_a_norm) @ w_uq).reshape(B, S, MLA_HEADS, MLA_QK)
    kv = (rmsnorm(c_kv, kv_a_norm) @ w_ukv).reshape(B, S, MLA_HEADS, MLA_NOPE + MLA_V)
    k = jnp.concatenate([kv[..., :MLA_NOPE],
                         jnp.broadcast_to(k_pe[:, :, None, :], (B, S, MLA_HEADS, MLA_ROPE))], axis=-1)
    v = kv[..., MLA_NOPE:]
    q = rmsnorm(q, q_norm)
    k = rmsnorm(k, k_norm)
    q = jnp.concatenate([q[..., :MLA_NOPE], rope(q[..., MLA_NOPE:], pos)], axis=-1)
    k = jnp.concatenate([k[..., :MLA_NOPE], rope(k[..., MLA_NOPE:], pos)], axis=-1)
    o = block_causal_attention(q, k, v)
    return o.reshape(B, S, MLA_HEADS * MLA_V)


def even_mixer(h, w_in, gm_v_norm, gm_w_s, gm_b_s, q_a_norm, w_uq, kv_a_norm, w_ukv,
               q_norm, k_norm, w_out, pos):
    z = h @ w_in
    o1 = GM_WIDTH
    o2 = 2 * GM_WIDTH
    o3 = o2 + MLA_Q_RANK
    o4 = o3 + MLA_KV_RANK
    u = jax.nn.gelu(z[..., :o1])
    v = jax.nn.gelu(z[..., o1:o2])
    a = spatial_gating(u, v, gm_v_norm, gm_w_s, gm_b_s)
    b = mla(z[..., o2:o3], z[..., o3:o4], z[..., o4:], q_a_norm, w_uq, kv_a_norm, w_ukv,
            q_norm, k_norm, pos)
    return jnp.concatenate([a, b], axis=-1) @ w_out


def retention(q, k, v):
    B, S, H, dk = q.shape
    dv = v.shape[-1]
    n = S // CHUNK
    lg = jnp.log(1.0 - 2.0 ** (-5.0 - jnp.arange(H, dtype=jnp.float32)))
    j = jnp.arange(CHUNK, dtype=jnp.float32)
    diff = j[:, None] - j[None, :]
    dmask = jnp.where(diff >= 0, jnp.exp(lg[:, None, None] * jnp.maximum(diff, 0.0)), 0.0)
    xi = jnp.exp(lg[:, None] * (j + 1.0))[None, :, :, None]
    zeta = jnp.exp(lg[:, None] * (CHUNK - 1.0 - j))[None, :, :, None]
    g_chunk = jnp.exp(lg * CHUNK)[None, :, None, None]

    def to_chunks(t):
        return t.astype(jnp.float32).reshape(B, n, CHUNK, H, -1).transpose(1, 0, 3, 2, 4)

    qc = to_chunks(q)
    kc = to_chunks(k) * (dk ** -0.5)
    vc = to_chunks(v)

    def step(state, inp):
        qi, ki, vi = inp
        a = jnp.einsum('bhjd,bhmd->bhjm', qi, ki) * dmask
        o = jnp.einsum('bhjm,bhme->bhje', a, vi) + jnp.einsum('bhjd,bhde->bhje', qi, state) * xi
        state = state * g_chunk + jnp.einsum('bhmd,bhme->bhde', ki * zeta, vi)
        return state, o

    state0 = jnp.zeros((B, H, dk, dv), jnp.float32)
    _, o = lax.scan(step, state0, (qc, kc, vc))
    return o.transpose(1, 0, 3, 2, 4).reshape(B, S, H, dv)


def odd_mixer(h, w_in, out_norm, w_out, pos):
    B, S, _ = h.shape
    nq = RET_HEADS * RET_QK
    nv = RET_HEADS * RET_V
    z = h @ w_in
    q = rope(z[..., :nq].reshape(B, S, RET_HEADS, RET_QK), pos)
    k = rope(z[..., nq:2 * nq].reshape(B, S, RET_HEADS, RET_QK), pos)
    v = z[..., 2 * nq:2 * nq + nv].reshape(B, S, RET_HEADS, RET_V)
    g = z[..., 2 * nq + nv:]
    o = rmsnorm(retention(q, k, v), out_norm)
    o = o.reshape(B, S, nv).astype(h.dtype) * jax.nn.silu(g)
    return o @ w_out


def swiglu(h, w_gate, w_up, w_down):
    return (jax.nn.silu(h @ w_gate) * (h @ w_up)) @ w_down


def setup_inputs(seed: int = 0) -> dict:
    key = jax.random.key(seed)
    ks = jax.random.split(key, 24)
    f32 = jnp.float32

    def w(k, shape, fan_in):
        return jax.random.normal(k, shape, f32) * (fan_in ** -0.5)

    def gain(k, shape):
        return 1.0 + 0.1 * jax.random.normal(k, shape, f32)

    return {
        "x": jax.random.normal(ks[0], (BATCH, SEQ, D_MODEL), f32),
        "norm_mix": gain(ks[1], (DEPTH, D_MODEL)),
        "norm_ffn": gain(ks[2], (DEPTH, D_MODEL)),
        "even_w_in": w(ks[3], (N_EVEN, D_MODEL, EVEN_IN), D_MODEL),
        "gm_v_norm": gain(ks[4], (N_EVEN, GM_GROUPS, GM_GROUP_DIM)),
        "gm_w_s": w(ks[5], (N_EVEN, GM_GROUPS, GM_BLOCK, GM_BLOCK), GM_BLOCK),
        "gm_b_s": gain(ks[6], (N_EVEN, GM_GROUPS, GM_BLOCK)),
        "mla_q_a_norm": gain(ks[7], (N_EVEN, MLA_Q_RANK)),
        "mla_w_uq": w(ks[8], (N_EVEN, MLA_Q_RANK, MLA_HEADS * MLA_QK), MLA_Q_RANK),
        "mla_kv_a_norm": gain(ks[9], (N_EVEN, MLA_KV_RANK)),
        "mla_w_ukv": w(ks[10], (N_EVEN, MLA_KV_RANK, MLA_HEADS * (MLA_NOPE + MLA_V)), MLA_KV_RANK),
        "mla_q_norm": gain(ks[11], (N_EVEN, MLA_QK)),
        "mla_k_norm": gain(ks[12], (N_EVEN, MLA_QK)),
        "even_w_out": w(ks[13], (N_EVEN, EVEN_MIX, D_MODEL), EVEN_MIX),
        "odd_w_in": w(ks[14], (N_ODD, D_MODEL, ODD_IN), D_MODEL),
        "ret_out_norm": gain(ks[15], (N_ODD, RET_HEADS, RET_V)),
        "odd_w_out": w(ks[16], (N_ODD, ODD_MIX, D_MODEL), ODD_MIX),
        "ffn_w_gate": w(ks[17], (DEPTH, D_MODEL, D_FF), D_MODEL),
        "ffn_w_up": w(ks[18], (DEPTH, D_MODEL, D_FF), D_MODEL),
        "ffn_w_down": w(ks[19], (DEPTH, D_FF, D_MODEL), D_FF),
    }


def reference(x, norm_mix, norm_ffn, even_w_in, gm_v_norm, gm_w_s, gm_b_s,
              mla_q_a_norm, mla_w_uq, mla_kv_a_norm, mla_w_ukv, mla_q_norm, mla_k_norm,
              even_w_out, odd_w_in, ret_out_norm, odd_w_out,
              ffn_w_gate, ffn_w_up, ffn_w_down):
    pos = jnp.arange(x.shape[1])
    for l in range(DEPTH):
        i = l // 2
        h = rmsnorm(x, norm_mix[l])
        if l % 2 == 0:
            mix = even_mixer(h, even_w_in[i], gm_v_norm[i], gm_w_s[i], gm_b_s[i],
                             mla_q_a_norm[i], mla_w_uq[i], mla_kv_a_norm[i], mla_w_ukv[i],
                             mla_q_norm[i], mla_k_norm[i], even_w_out[i], pos)
        else:
            mix = odd_mixer(h, odd_w_in[i], ret_out_norm[i], odd_w_out[i], pos)
        x = x + mix
        h = rmsnorm(x, norm_ffn[l])
        x = x + swiglu(h, ffn_w_gate[l], ffn_w_up[l], ffn_w_down[l])
    return x
```

```python
import math
from contextlib import ExitStack
import numpy as np
import ml_dtypes
import concourse.bass as bass
import concourse.mybir as mybir
from concourse.bass_utils import run_bass_kernel_spmd

F32 = mybir.dt.float32
BF16 = mybir.dt.bfloat16
AF = mybir.ActivationFunctionType
ALU = mybir.AluOpType

D = 1024
EIN = 1696
DFF = 2816
OIN = 6144
EPS = 1e-6
NCORES = 4


class Dep:
    __slots__ = ("w", "r", "name", "psum")

    def __init__(self, name="", psum=False):
        self.w = None
        self.r = []
        self.name = name
        self.psum = psum


class Sched:
    ENG = ("pe", "act", "dve", "pool", "sp")

    def __init__(self, nc, stack, sem_limit=28000):
        self.nc = nc
        self.stack = stack
        self.e = {"pe": nc.tensor, "act": nc.scalar, "dve": nc.vector,
                  "pool": nc.gpsimd, "sp": nc.sync}
        self.sem_limit = sem_limit
        self.sem = {}
        self.cnt = {}
        self.nsem = 0
        self.pending = {}
        for k in ("pe", "act", "dve", "pool"):
            self._new_sem(k)
        self.seen = {k: {} for k in self.ENG}
        self.n_wait = 0
        self.n_ops = 0
        self.all_tickets = {}
        self.free_dma = {}
        self.live_dma = []

    def _alloc_sem(self, name):
        self.nsem += 1
        return self.stack.enter_context(self.nc.semaphore(f"{name}_{self.nsem}"))

    def _new_sem(self, k):
        self.sem[k] = self._alloc_sem("s" + k)
        self.cnt[k] = 0

    def new_dma_sem(self, kind="hw"):
        fl = self.free_dma.setdefault(kind, [])
        s = fl.pop() if fl else [self._alloc_sem("sdma" + kind), 0]
        self.live_dma.append((kind, s))
        return s

    def release_dma_sems(self):
        for kind, s in self.live_dma:
            self.free_dma[kind].append(s)
        self.live_dma = []

    def _wait_for(self, e, reads, writes):
        need = {}

        def add(t):
            if t is None:
                return
            sem, val = t
            k = id(sem)
            if k not in need or need[k][1] < val:
                need[k] = (sem, val)
        for b in reads:
            add(b.w)
            if b.psum:
                for t in b.r:
                    if t[0] is not self.sem.get(e):
                        add(t)
        for b in writes:
            add(b.w)
            for t in b.r:
                add(t)
        eng = self.e[e]
        seen = self.seen[e]
        for k, (sem, val) in need.items():
            if e == "pe" and sem is self.sem.get("pe"):
                continue
            if seen.get(k, 0) < val:
                eng.wait_ge(sem, val)
                seen[k] = val
                self.n_wait += 1

    def _record(self, t, reads, writes):
        self.all_tickets[id(t[0])] = t if (id(t[0]) not in self.all_tickets or self.all_tickets[id(t[0])][1] < t[1]) else self.all_tickets[id(t[0])]
        for b in reads:
            b.r.append(t)
            if len(b.r) > 16:
                best = {}
                for sem, val in b.r:
                    k = id(sem)
                    if k not in best or best[k][1] < val:
                        best[k] = (sem, val)
                b.r = list(best.values())
        for b in writes:
            b.w = t
            b.r = []

    def op(self, e, fn, reads=(), writes=(), inc=True):
        self._wait_for(e, reads, writes)
        ins = fn(self.e[e])
        self.n_ops += 1
        if self.cnt[e] >= self.sem_limit and not self.pending.get(e):
            self._new_sem(e)
        self.pending[e] = not inc
        if inc:
            self.cnt[e] += 1
            ins.then_inc(self.sem[e], 1)
            t = (self.sem[e], self.cnt[e])
        else:
            t = (self.sem[e], self.cnt[e] + 1)
        self._record(t, reads, writes)
        return ins

    def dma(self, q, semslot, outs_ins, reads=(), writes=()):
        if semslot[1] >= self.sem_limit:
            semslot[0] = self._alloc_sem("sdma")
            semslot[1] = 0
        self._wait_for(q, reads, writes)
        eng = self.e[q]
        for (o, i) in outs_ins:
            eng.dma_start(out=o, in_=i).then_inc(semslot[0], 16)
            semslot[1] += 16
        t = (semslot[0], semslot[1])
        self._record(t, reads, writes)

    def barrier(self):
        for e in self.ENG:
            eng = self.e[e]
            seen = self.seen[e]
            for k, (sem, val) in list(self.all_tickets.items()):
                if seen.get(k, 0) < val:
                    eng.wait_ge(sem, val)
                    seen[k] = val


class Pool:
    def __init__(self, kb, name, n, shape, dtype, space="sbuf", dma=False):
        self.n = n
        self.i = 0
        self.tiles = []
        for j in range(n):
            if space == "sbuf":
                t = kb.st.enter_context(kb.nc.sbuf_tensor(f"{name}{j}_{kb.uid()}", list(shape), dtype))
            else:
                t = kb.st.enter_context(kb.nc.psum_tensor(f"{name}{j}_{kb.uid()}", list(shape), dtype))
            self.tiles.append((t, Dep(f"{name}{j}", psum=(space == "psum")), kb.S.new_dma_sem() if dma else None))

    def next(self):
        r = self.tiles[self.i % self.n]
        self.i += 1
        return r


_UID = [0]
_SSEM = {}


class KB:
    def __init__(self, nc, S, st):
        self.nc = nc
        self.S = S
        self.st = st
        self._uid = 0
        self.ssem = {}

    def uid(self):
        _UID[0] += 1
        return _UID[0]

    def tile(self, name, shape, dt, dma=False):
        t = self.st.enter_context(self.nc.sbuf_tensor(f"{name}_{self.uid()}", list(shape), dt))
        if dma:
            return t, Dep(name), self.S.new_dma_sem()
        return t, Dep(name)

    def sub(self, st):
        k = KB(self.nc, self.S, st)
        k._uid = self._uid + 1000
        return k

    def mm(self, out, od, pairs, reads):
        n = len(pairs)
        for i, (l, r) in enumerate(pairs):
            self.S.op("pe", lambda e, l=l, r=r, i=i: e.matmul(out, lhsT=l, rhs=r, start=(i == 0), stop=(i == n - 1)),
                      reads=reads, writes=[od], inc=(i == n - 1))

    def mm1(self, out, od, l, r, start, stop, reads, inc=True):
        self.S.op("pe", lambda e: e.matmul(out, lhsT=l, rhs=r, start=start, stop=stop),
                  reads=reads, writes=[od], inc=inc)

    def tr(self, out, od, in_, ident, reads, inc=True):
        self.S.op("pe", lambda e: e.transpose(out=out, in_=in_, identity=ident), reads=reads, writes=[od], inc=inc)

    def act(self, out, in_, func, reads, writes, **kw):
        self.S.op("act", lambda e: e.activation(out=out, in_=in_, func=func, **kw), reads=reads, writes=writes)

    def tt(self, eng, out, in0, in1, op, reads, writes):
        self.S.op(eng, lambda e: e.tensor_tensor(out=out, in0=in0, in1=in1, op=op), reads=reads, writes=writes)

    def ts(self, eng, out, in0, s1, s2, op0, op1, reads, writes):
        if s2 is None:
            self.S.op(eng, lambda e: e.tensor_scalar(out=out, in0=in0, scalar1=s1, scalar2=None, op0=op0), reads=reads, writes=writes)
        else:
            self.S.op(eng, lambda e: e.tensor_scalar(out=out, in0=in0, scalar1=s1, scalar2=s2, op0=op0, op1=op1), reads=reads, writes=writes)

    def stt(self, eng, out, in0, scalar, in1, op0, op1, reads, writes):
        self.S.op(eng, lambda e: e.scalar_tensor_tensor(out=out, in0=in0, scalar=scalar, in1=in1, op0=op0, op1=op1),
                  reads=reads, writes=writes)

    def cp(self, eng, out, in_, reads, writes):
        if eng == "act":
            self.S.op("act", lambda e: e.copy(out=out, in_=in_), reads=reads, writes=writes)
        else:
            self.S.op(eng, lambda e: e.tensor_copy(out=out, in_=in_), reads=reads, writes=writes)

    def recip(self, out, in_, reads, writes):
        self.S.op("dve", lambda e: e.reciprocal(out=out, in_=in_), reads=reads, writes=writes)

    def memset(self, eng, ap, val, writes):
        self.S.op(eng, lambda e: e.memset(ap, val), reads=[], writes=writes)

    def rstd(self, out, in_, scale, epst, reads, writes):
        self.act(out, in_, AF.Ln, reads, writes, scale=scale, bias=epst)
        self.act(out, out, AF.Exp, writes, writes, scale=-0.5)

    def load(self, q, sem, out, in_, od, reads=()):
        self.S.dma(q, sem, [(out, in_)], reads=list(reads), writes=[od])

    def store(self, q, sem, out, in_, src_dep, dram_dep):
        k = id(sem)
        if k not in self.ssem:
            self.ssem[k] = (sem, self.S.new_dma_sem("sw"))
        self.S.dma(q, self.ssem[k][1], [(out, in_)], reads=[src_dep], writes=[dram_dep])


def load_cast_weight(kb, stage_pool, dst, dst_dep, src_ap_fn, nchunks, ncols, engs=("dve", "pool"), p0=0, np_=None):
    P = dst.shape[0] if np_ is None else np_
    CW = stage_pool.tiles[0][0].shape[1]
    i = 0
    for c in range(nchunks):
        for c0 in range(0, ncols, CW):
            w = min(CW, ncols - c0)
            stg, sd, ssem = stage_pool.next()
            kb.load("sp", ssem, stg[p0:p0 + P, 0:w], src_ap_fn(c, c0, w), sd)
            kb.cp(engs[i % len(engs)], dst[p0:p0 + P, c, c0:c0 + w], stg[p0:p0 + P, 0:w], [sd], [dst_dep])
            i += 1


def norm_transpose(kb, C, xt, xd, gb, gbd, hT, hTd, col0, pools):
    ss, ssd = pools["ss"].next()[:2]
    hn, hnd = pools["hn"].next()[:2]
    junk, junkd = pools["junk"] if pools.get("junk") is not None else (hn, hnd)
    kb.act(junk[:, :], xt[:, :], AF.Square, [xd], [junkd, ssd], accum_out=ss[:, 0:1])
    kb.rstd(ss[:, 0:1], ss[:, 0:1], 1.0 / D, C["eps"][0][:, 0:1], [ssd, C["eps"][1]], [ssd])
    kb.stt("dve", hn[:, :], xt[:, :], ss[:, 0:1], gb[:, :], ALU.mult, ALU.mult, [xd, ssd, gbd], [hnd])
    pT, pTd = pools["psT"].next()[:2]
    for c in range(8):
        kb.tr(pT[:, c * 128:(c + 1) * 128], pTd, hn[:, c * 128:(c + 1) * 128], C["ident"][0][:, :],
              [hnd, C["ident"][1]], inc=(c == 7))
    kb.cp("act", hT[:, :, col0:col0 + 128], pT[:, :].rearrange("p (c t) -> p c t", c=8), [pTd], [hTd])


def load_consts(kb, dr):
    C = {}
    for name, shape, dt in (("ident", [128, 128], BF16), ("ones", [128, 128], BF16),
                            ("eps", [128, 1], F32)):
        t, d, s = kb.tile(name, shape, dt, dma=True)
        kb.load("sp", s, t[:, :], dr[name][:, :], d)
        C[name] = (t, d)
    return C


import os
STOP = int(os.environ.get('KSTOP', '99'))


def phase_a1(nc, S, dr, SEQ):
    with ExitStack() as st:
        kb = KB(nc, S, st)
        C = load_consts(kb, dr)
        TS = 512
        NST = SEQ // TS
        w_in, w_ind = kb.tile("w_in", [128, 8, EIN], BF16)
        wsT, wsTd = kb.tile("wsT", [128, 4, 128], BF16)
        stage = Pool(kb, "stg", 5, [128, 1024], F32, dma=True)
        gmix, gmixd, gs = kb.tile("gmix", [128, D], F32, dma=True)
        kb.load("sp", gs, gmix[:, :], dr["gmix0"][:, :], gmixd)
        gvn, gvnd, gs = kb.tile("gvn", [128, 512], F32, dma=True)
        kb.load("sp", gs, gvn[:, :], dr["gvn"][:, :], gvnd)
        bsb, bsbd, gs = kb.tile("bsb", [128, 512], F32, dma=True)
        kb.load("sp", gs, bsb[:, :], dr["bsb"][:, :], bsbd)
        gqa, gqad, gs = kb.tile("gqa", [128, 3], F32, dma=True)
        kb.load("sp", gs, gqa[:, :], dr["gqa"][:, :], gqad)
        gkva, gkvad, gs = kb.tile("gkva", [128, 2], F32, dma=True)
        kb.load("sp", gs, gkva[:, :], dr["gkva"][:, :], gkvad)
        load_cast_weight(kb, stage, w_in, w_ind, lambda c, c0, w: dr["w_in0"][:, c, c0:c0 + w], 8, EIN)
        load_cast_weight(kb, stage, wsT, wsTd, lambda c, c0, w: dr["wsT"][:, c, c0:c0 + w], 4, 128)
        for g in range(4):
            kb.memset("pool", wsT[64:128, g, 0:64], 0.0, [wsTd])

        if STOP <= 0:
            S.barrier(); S.release_dma_sems(); return
        xin = Pool(kb, "xin", 3, [128, D], F32, dma=True)
        junk = kb.tile("junk", [128, D], BF16)
        pools = {"junk": junk, "ss": Pool(kb, "ss", 3, [128, 4], F32), "hn": Pool(kb, "hn", 2, [128, D], BF16),
                 "psT": Pool(kb, "psT", 2, [128, 1024], BF16, space="psum")}
        ps = Pool(kb, "ps", 4, [128, 512], F32, space="psum")
        hTp = Pool(kb, "hT", 2, [128, 8, TS], BF16)
        uTp = Pool(kb, "uT", 1, [128, 4, TS], F32)
        cfp = Pool(kb, "cf", 2, [128, 3, TS], F32)
        sqp = Pool(kb, "sq", 2, [128, 3, TS], BF16)
        rsp = Pool(kb, "rs", 2, [128, TS], F32)
        cnp = Pool(kb, "cn", 2, [128, 3, TS], BF16, dma=True)
        kpp = Pool(kb, "kp", 2, [96, TS], F32, dma=True)
        vp = Pool(kb, "v", 2, [128, 512], F32)
        vnp = Pool(kb, "vn", 2, [128, 512], BF16)
        tmpp = Pool(kb, "tmp", 2, [128, 512], F32)
        mixp = Pool(kb, "mixa", 2, [128, 4, TS], BF16, dma=True)
        eps = C["eps"]
        ones = C["ones"]

        for stile in range(NST):
            t0 = stile * TS
            hT, hTd, _ = hTp.next()
            for sub in range(4):
                xt, xd, xs = xin.next()
                kb.load("sp", xs, xt[:, :], dr["x"][t0 + sub * 128:t0 + (sub + 1) * 128, :], xd)
                norm_transpose(kb, C, xt, xd, gmix, gmixd, hT, hTd, sub * 128, pools)
            if STOP <= 1:
                S.barrier(); S.release_dma_sems(); return
            uT, uTd, _ = uTp.next()
            for g in range(4):
                p, pd, _ = ps.next()
                kb.mm(p[:, :], pd, [(w_in[:, k, g * 128:(g + 1) * 128], hT[:, k, :]) for k in range(8)], [w_ind, hTd])
                kb.act(uT[:, g, :], p[:, :], AF.Gelu_apprx_tanh, [pd], [uTd])
            if STOP <= 2:
                S.barrier(); S.release_dma_sems(); return
            mixa, mixad, mixs = mixp.next()
            for sub in range(4):
                p, pd, _ = ps.next()
                kb.mm(p[:, :], pd, [(hT[:, k, sub * 128:(sub + 1) * 128], w_in[:, k, 512:1024]) for k in range(8)],
                      [w_ind, hTd])
                v, vd, _ = vp.next()
                kb.act(v[:, :], p[:, :], AF.Gelu_apprx_tanh, [pd], [vd])
                ss, ssd, _ = pools["ss"].next()
                jk, jkd = junk
                for g in range(4):
                    kb.act(jk[:, 0:128], v[:, g * 128:(g + 1) * 128], AF.Square, [vd], [jkd, ssd],
                           accum_out=ss[:, g:g + 1])
                kb.rstd(ss[:, 0:4], ss[:, 0:4], 1.0 / 128, eps[0][:, 0:1], [ssd, eps[1]], [ssd])
                vn, vnd, _ = vnp.next()
                for g in range(4):
                    kb.stt("dve", vn[:, g * 128:(g + 1) * 128], v[:, g * 128:(g + 1) * 128], ss[:, g:g + 1],
                           gvn[:, g * 128:(g + 1) * 128], ALU.mult, ALU.mult, [vd, ssd, gvnd], [vnd])
                p2, p2d, _ = ps.next()
                for g in range(4):
                    kb.mm(p2[:, g * 128:(g + 1) * 128], p2d, [(vn[:, g * 128:(g + 1) * 128], wsT[:, g, :])],
                          [vnd, wsTd])
                tmp, tmpd, _ = tmpp.next()
                kb.tt("dve", tmp[:, :], p2[:, :], bsb[:, :], ALU.add, [p2d, bsbd], [tmpd])
                kb.tt("pool", mixa[:, :, sub * 128:(sub + 1) * 128], tmp[:, :].rearrange("p (g t) -> p g t", g=4),
                      uT[:, :, sub * 128:(sub + 1) * 128], ALU.mult, [tmpd, uTd], [mixad])
            for g in range(4):
                kb.store("pool", mixs, dr["mixa_s"][g, :, t0:t0 + TS], mixa[:, g, :], mixad, dr["_d_mixa"][stile])
            if STOP <= 3:
                S.barrier(); S.release_dma_sems(); return
            for (nch, col, gt, gtd, dst, ddep, nfeat) in ((3, 1024, gqa, gqad, "cqn_s", "_d_cqn", 384),
                                                          (2, 1408, gkva, gkvad, "ckvn_s", "_d_ckvn", 256)):
                cf, cfd, _ = cfp.next()
                sq, sqd, _ = sqp.next()
                for c in range(nch):
                    p, pd, _ = ps.next()
                    kb.mm(p[:, :], pd, [(w_in[:, k, col + c * 128:col + (c + 1) * 128], hT[:, k, :]) for k in range(8)],
                          [w_ind, hTd])
                    kb.cp("dve", cf[:, c, :], p[:, :], [pd], [cfd])
                    kb.act(sq[:, c, :], p[:, :], AF.Square, [pd], [sqd])
                p, pd, _ = ps.next()
                kb.mm(p[:, :], pd, [(ones[0][:, :], sq[:, c, :]) for c in range(nch)], [ones[1], sqd])
                rs, rsd, _ = rsp.next()
                kb.rstd(rs[:, :], p[:, :], 1.0 / nfeat, eps[0][:, 0:1], [pd, eps[1]], [rsd])
                cn, cnd, cns = cnp.next()
                for c in range(nch):
                    kb.stt("dve", cn[:, c, :], cf[:, c, :], gt[:, c:c + 1], rs[:, :], ALU.mult, ALU.mult,
                           [cfd, gtd, rsd], [cnd])
                for c in range(nch):
                    if os.environ.get("KSKIP") == "cst":
                        continue
                    kb.store("pool", cns, dr[dst][c, :, t0:t0 + TS], cn[:, c, :], cnd, dr[ddep][stile])
            if STOP <= 4:
                S.barrier(); S.release_dma_sems(); return
            p, pd, _ = ps.next()
            kb.mm(p[64:96, :], pd, [(w_in[:, k, 1664:1696], hT[:, k, :]) for k in range(8)], [w_ind, hTd])
            kp, kpd, kps = kpp.next()
            kb.cp("dve", kp[64:96, :], p[64:96, :], [pd], [kpd])
            kb.store("pool", kps, dr["kpe_s"][:, t0:t0 + TS], kp[64:96, :], kpd, dr["_d_kpe"][stile])
        if os.environ.get('KDEBUG'):
            print('sbuf remaining', nc.sbuf_bytes_remaining)
        S.barrier()
        S.release_dma_sems()


def phase_a2(nc, S, dr, SEQ):
    with ExitStack() as st:
        kb = KB(nc, S, st)
        C = load_consts(kb, dr)
        ones, eps = C["ones"], C["eps"]
        TS = 256
        NST = SEQ // TS
        NT = SEQ // 128
        SCALE = 96.0 ** -0.5
        stage = Pool(kb, "stg", 2, [128, 768], F32, dma=True)
        w_uq, w_uqd = kb.tile("w_uq", [128, 3, 768], BF16)
        w_ukv, w_ukvd = kb.tile("w_ukv", [128, 2, 1024], BF16)
        w_oa, w_oad = kb.tile("w_oa", [128, 4, D], BF16)
        w_ob, w_obd = kb.tile("w_ob", [128, 4, D], BF16)
        load_cast_weight(kb, stage, w_uq, w_uqd, lambda c, c0, w: dr["w_uq"][:, c, c0:c0 + w], 3, 768)
        load_cast_weight(kb, stage, w_ukv, w_ukvd, lambda c, c0, w: dr["w_ukv"][:, c, c0:c0 + w], 2, 1024)
        load_cast_weight(kb, stage, w_oa, w_oad, lambda c, c0, w: dr["w_oa"][:, c, c0:c0 + w], 4, D)
        load_cast_weight(kb, stage, w_ob, w_obd, lambda c, c0, w: dr["w_ob"][:, c, c0:c0 + w], 4, D)
        gq, gqd, gs = kb.tile("gq", [96, 1], F32, dma=True)
        kb.load("sp", gs, gq[:, :], dr["gq"][:, :], gqd)
        gk, gkd, gs = kb.tile("gk", [96, 1], F32, dma=True)
        kb.load("sp", gs, gk[:, :], dr["gk"][:, :], gkd)
        pf, pfd, gs = kb.tile("pfull", [96, 96], BF16, dma=True)
        kb.load("sp", gs, pf[:, :], dr["pfull"][:, :], pfd)
        Kc = [kb.tile(f"Kc{h}", [96, SEQ], BF16)[0] for h in range(8)]
        Kdep = [Dep(f"K{i}") for i in range(NST)]
        Vc, _ = kb.tile("Vc", [128, NT, 4, 192], BF16)
        Vdep = [Dep(f"V{i}") for i in range(NT)]
        Vones = Dep("Vones")
        for pr in range(4):
            kb.memset("pool", Vc[:, :, pr, 64:128], 1.0, [Vones])
        id32, id32d, gs = kb.tile("id32", [128, 64], F32, dma=True)
        kb.load("sp", gs, id32[:, :], dr["ident32x2"][:, :], id32d)

        def vlhsT(kt, h):
            return Vc[:, kt, h // 2, 0:128] if h % 2 == 0 else Vc[:, kt, h // 2, 64:192]

        cqp = Pool(kb, "cq", 2, [128, 3, TS], BF16, dma=True)
        ckp = Pool(kb, "ck", 2, [128, 2, TS], BF16, dma=True)
        kpp = Pool(kb, "kp", 2, [96, TS], F32, dma=True)
        cosp = Pool(kb, "cos", 2, [96, TS], F32, dma=True)
        sinp = Pool(kb, "sin", 2, [96, TS], F32, dma=True)
        mixp = Pool(kb, "mixa", 2, [128, 4, TS], BF16, dma=True)
        xin = Pool(kb, "xin", 3, [128, D], F32, dma=True)
        psK = Pool(kb, "psK", 2, [128, 512], F32, space="psum")
        psQ = Pool(kb, "psQ", 2, [128, 512], F32, space="psum")
        pSp = Pool(kb, "pS", 3, [128, 512], F32, space="psum")
        pop = Pool(kb, "po", 1, [128, 512], F32, space="psum")
        sqpe, sqped = kb.tile("sqpe", [96, TS], BF16)
        sspe, ssped = kb.tile("sspe", [96, TS], F32)
        kpg, kpgd = kb.tile("kpg", [96, TS], F32)
        kpgb, kpgbd = kb.tile("kpgb", [96, TS], BF16)
        kper, kperd = kb.tile("kper", [96, TS], F32)
        t1p = Pool(kb, "t1", 2, [96, TS], F32)
        t1K = Pool(kb, "t1K", 1, [96, TS], F32)
        t2K = Pool(kb, "t2K", 1, [96, TS], F32)
        t2p = Pool(kb, "t2", 2, [96, TS], F32)
        sqK = Pool(kb, "sqK", 2, [96, TS], BF16)
        sqQ = Pool(kb, "sqQ", 2, [96, TS], BF16)
        rkK = Pool(kb, "rkK", 2, [96, TS], F32)
        rkQ = Pool(kb, "rkQ", 2, [96, TS], F32)
        qnp = Pool(kb, "qn", 2, [96, TS], F32)
        qnbp = Pool(kb, "qnb", 2, [96, TS], BF16)
        Qhp = Pool(kb, "Qh", 16, [96, TS], BF16)
        PTp = Pool(kb, "PT", 4, [128, TS], BF16)
        recp = Pool(kb, "rec", 2, [128, TS], F32)
        recsp = Pool(kb, "recs", 2, [128, TS], F32)
        obp = Pool(kb, "ob", 2, [128, 4, TS], BF16)

        PREP = {}

        def rr(*gens):
            gens = list(gens)
            while gens:
                for g_ in list(gens):
                    try:
                        next(g_)
                        yield
                    except StopIteration:
                        gens.remove(g_)

        def prep(stile):
            L = {}
            yield from prep_loads(stile, L)
            yield from rr(prep_k(stile, L), prep_vq(stile, L))

        def prep_loads(stile, L):
            t0 = stile * TS
            cq, cqd, cqs = cqp.next()
            kb.load("sp", cqs, cq[:, :, :], dr["cqn_s"][:, :, t0:t0 + TS].rearrange("c p t -> p c t"), cqd,
                    reads=[dr["_d_cqn"][t0 // 512]])
            ck, ckd, cks = ckp.next()
            kb.load("sp", cks, ck[:, :, :], dr["ckvn_s"][:, :, t0:t0 + TS].rearrange("c p t -> p c t"), ckd,
                    reads=[dr["_d_ckvn"][t0 // 512]])
            kp, kpd, kps = kpp.next()
            kb.load("sp", kps, kp[64:96, :], dr["kpe_s"][:, t0:t0 + TS], kpd, reads=[dr["_d_kpe"][t0 // 512]])
            cs, csd, css = cosp.next()
            kb.load("sp", css, cs[64:96, :], dr["cosM"][:, t0:t0 + TS], csd)
            sn, snd, sns = sinp.next()
            kb.load("sp", sns, sn[64:96, :], dr["sinM"][:, t0:t0 + TS], snd)
            L.update(cq=cq, cqd=cqd, ck=ck, ckd=ckd, kp=kp, kpd=kpd, cs=cs, csd=csd, sn=sn, snd=snd)
            yield

        def prep_k(stile, L):
            t0 = stile * TS
            ck, ckd, kp, kpd, cs, csd, sn, snd = (L[k_] for k_ in ("ck", "ckd", "kp", "kpd", "cs", "csd", "sn", "snd"))
            kb.act(sqpe[64:96, :], kp[64:96, :], AF.Square, [kpd], [sqped])
            kb.ts("dve", kpg[64:96, :], kp[64:96, :], gk[64:96, 0:1], None, ALU.mult, None, [kpd, gkd], [kpgd])
            yield
            kb.cp("pool", kpgb[64:96, :], kpg[64:96, :], [kpgd], [kpgbd])
            yield
            p, pd, _ = psK.next()
            kb.mm(p[0:96, 0:TS], pd, [(pf[64:96, 0:96], kpgb[64:96, :])], [pfd, kpgbd])
            yield
            t1, t1d, _ = t1K.next()
            kb.tt("dve", t1[64:96, :], p[64:96, 0:TS], sn[64:96, :], ALU.mult, [pd, snd], [t1d])
            t2, t2d, _ = t2K.next()
            kb.tt("pool", t2[64:96, :], kpg[64:96, :], cs[64:96, :], ALU.mult, [kpgd, csd], [t2d])
            yield
            kb.tt("pool", kper[64:96, :], t1[64:96, :], t2[64:96, :], ALU.add, [t1d, t2d], [kperd])
            p, pd, _ = psK.next()
            kb.mm(p[0:96, 0:TS], pd, [(ones[0][64:96, 0:96], sqpe[64:96, :])], [ones[1], sqped])
            yield
            kb.cp("dve", sspe[:, :], p[0:96, 0:TS], [pd], [ssped])
            yield
            for h in range(8):
                p, pd, _ = psK.next()
                kb.mm(p[0:64, 0:TS], pd, [(w_ukv[:, c, h * 128:h * 128 + 64], ck[:, c, :]) for c in range(2)],
                      [w_ukvd, ckd])
                yield
                sq, sqd, _ = sqK.next()
                kb.act(sq[0:64, :], p[0:64, 0:TS], AF.Square, [pd], [sqd])
                yield
                p2, p2d, _ = psK.next()
                kb.mm(p2[0:96, 0:TS], p2d, [(ones[0][0:64, 0:96], sq[0:64, :])], [ones[1], sqd])
                yield
                rk, rkd, _ = rkK.next()
                kb.tt("dve", rk[:, :], p2[0:96, 0:TS], sspe[:, :], ALU.add, [p2d, ssped], [rkd])
                yield
                kb.act(rk[:, :], rk[:, :], AF.Ln, [rkd, eps[1]], [rkd], scale=1.0 / 96, bias=eps[0][0:96, 0:1])
                yield
                kb.act(rk[:, :], rk[:, :], AF.Exp, [rkd], [rkd], scale=-0.5)
                yield
                kb.stt("dve", Kc[h][0:64, t0:t0 + TS], p[0:64, 0:TS], gk[0:64, 0:1], rk[0:64, :], ALU.mult, ALU.mult,
                       [pd, gkd, rkd], [Kdep[stile]])
                kb.tt("pool", Kc[h][64:96, t0:t0 + TS], kper[64:96, :], rk[64:96, :], ALU.mult,
                      [kperd, rkd], [Kdep[stile]])
                yield

        def prep_vq(stile, L):
            t0 = stile * TS
            cq, cqd, ck, ckd, cs, csd, sn, snd = (L[k_] for k_ in ("cq", "cqd", "ck", "ckd", "cs", "csd", "sn", "snd"))
            for sub in range(TS // 128):
                ti = t0 // 128 + sub
                p, pd, _ = psQ.next()
                kb.mm(p[:, :].rearrange("p (h e) -> p h e", h=8), pd,
                      [(ck[:, c, sub * 128:(sub + 1) * 128],
                        w_ukv[:, c, :].rearrange("p (h e) -> p h e", h=8)[:, :, 64:128]) for c in range(2)],
                      [w_ukvd, ckd])
                yield
                pv4 = p[:, :].rearrange("p (q two e) -> p q two e", q=4, two=2)
                kb.cp("act", Vc[:, ti, :, 0:64], pv4[:, :, 0, :], [pd], [Vdep[ti]])
                yield
                kb.cp("act", Vc[:, ti, :, 128:192], pv4[:, :, 1, :], [pd], [Vdep[ti]])
                yield
            Qs = []
            for h in range(8):
                p, pd, _ = psQ.next()
                kb.mm(p[0:96, 0:TS], pd, [(w_uq[:, c, h * 96:(h + 1) * 96], cq[:, c, :]) for c in range(3)],
                      [w_uqd, cqd])
                yield
                sq, sqd, _ = sqQ.next()
                kb.act(sq[0:96, :], p[0:96, 0:TS], AF.Square, [pd], [sqd])
                yield
                p2, p2d, _ = psQ.next()
                kb.mm(p2[0:96, 0:TS], p2d, [(ones[0][0:96, 0:96], sq[0:96, :])], [ones[1], sqd])
                yield
                rq, rqd, _ = rkQ.next()
                kb.act(rq[:, :], p2[0:96, 0:TS], AF.Ln, [p2d, eps[1]], [rqd], scale=1.0 / 96, bias=eps[0][0:96, 0:1])
                yield
                kb.act(rq[:, :], rq[:, :], AF.Exp, [rqd], [rqd], scale=-0.5)
                yield
                qn, qnd, _ = qnp.next()
                kb.stt("dve", qn[:, :], p[0:96, 0:TS], gq[:, 0:1], rq[:, :], ALU.mult, ALU.mult, [pd, gqd, rqd], [qnd])
                yield
                qnb, qnbd, _ = qnbp.next()
                kb.cp("pool", qnb[64:96, :], qn[64:96, :], [qnd], [qnbd])
                yield
                p3, p3d, _ = psQ.next()
                kb.mm(p3[0:96, 0:TS], p3d, [(pf[64:96, 0:96], qnb[64:96, :])], [pfd, qnbd])
                Qh, Qhd, _ = Qhp.next()
                kb.cp("pool", Qh[0:64, :], qn[0:64, :], [qnd], [Qhd])
                yield
                t1, t1d, _ = t1p.next()
                kb.tt("dve", t1[64:96, :], p3[64:96, 0:TS], sn[64:96, :], ALU.mult, [p3d, snd], [t1d])
                t2, t2d, _ = t2p.next()
                kb.tt("pool", t2[64:96, :], qn[64:96, :], cs[64:96, :], ALU.mult, [qnd, csd], [t2d])
                yield
                kb.tt("pool", Qh[64:96, :], t1[64:96, :], t2[64:96, :], ALU.add, [t1d, t2d], [Qhd])
                Qs.append((Qh, Qhd))
                yield
            PREP[stile] = Qs

        NPREP = 175
        LOOK = 2
        for _ in prep(0):
            pass
        for stile in range(NST):
            t0 = stile * TS
            nq = TS // 128
            nkt = nq * (stile + 1)
            g = prep(stile + 1) if stile + 1 < NST else None
            spi = -(-NPREP // (8 * nkt))
            Qs = PREP.pop(stile)
            ob, obd, _ = obp.next()
            for h in range(8):
                Qh, Qhd = Qs[h]
                po, pod, _ = pop.next()

                def issue_qk(kt):
                    j = kt - nq * stile
                    c0 = 128 * j if j > 0 else 0
                    pS, pSd, _ = pSp.next()
                    kb.mm(pS[:, c0:TS], pSd, [(Kc[h][0:96, kt * 128:(kt + 1) * 128], Qh[0:96, c0:TS])],
                          [Kdep[(kt * 128) // TS], Qhd])
                    return pS, pSd, j, c0
                pend = [issue_qk(kt) for kt in range(min(LOOK, nkt))]
                for kt in range(nkt):
                    if kt + LOOK < nkt:
                        pend.append(issue_qk(kt + LOOK))
                    pS, pSd, j, c0 = pend.pop(0)
                    PT, PTd, _ = PTp.next()
                    kb.act(PT[:, c0:TS], pS[:, c0:TS], AF.Exp, [pSd], [PTd], scale=SCALE)
                    if j >= 0:
                        kb.memset("pool", PT[64:128, c0:c0 + 64], 0.0, [PTd])
                    first, last = (kt == 0), (kt == nkt - 1)
                    kb.mm1(po[:, c0:TS], pod, vlhsT(kt, h), PT[:, c0:TS], first, last,
                           [Vdep[kt], Vones, PTd], inc=True)
                    if g is not None:
                        for _ in range(spi):
                            next(g, None)
                o_sl, d_sl = (slice(0, 64), slice(64, 128)) if h % 2 == 0 else (slice(64, 128), slice(0, 64))
                rec, recd, _ = recp.next()
                kb.recip(rec[d_sl, :], po[d_sl, 0:TS], [pod], [recd])
                psh, pshd, _ = pSp.next()
                kb.mm(psh[o_sl, 0:TS], pshd, [(id32[d_sl, 0:64], rec[d_sl, :])], [id32d, recd])
                recs, recsd, _ = recsp.next()
                kb.cp("act", recs[o_sl, :], psh[o_sl, 0:TS], [pshd], [recsd])
                kb.tt("dve", ob[o_sl, h // 2, :], po[o_sl, 0:TS], recs[o_sl, :], ALU.mult, [pod, recsd], [obd])
            if g is not None:
                for _ in g:
                    pass
            mixa, mixad, mixs = mixp.next()
            kb.load("sp", mixs, mixa[:, :, :], dr["mixa_s"][:, :, t0:t0 + TS].rearrange("g p t -> p g t"), mixad,
                    reads=[dr["_d_mixa"][t0 // 512]])
            for sub in range(TS // 128):
                ti = t0 // 128 + sub
                xt, xd, xs = xin.next()
                kb.load("sp", xs, xt[:, :], dr["x"][ti * 128:(ti + 1) * 128, :], xd)
                for half in range(2):
                    p, pd, _ = pSp.next()
                    hs = slice(half * 512, (half + 1) * 512)
                    kb.mm(p[:, :], pd,
                          [(mixa[:, g, sub * 128:(sub + 1) * 128], w_oa[:, g, hs]) for g in range(4)] +
                          [(ob[:, pr, sub * 128:(sub + 1) * 128], w_ob[:, pr, hs]) for pr in range(4)],
                          [mixad, w_oad, obd, w_obd])
                    kb.tt("dve", xt[:, hs], xt[:, hs], p[:, :], ALU.add, [xd, pd], [xd])
                kb.store("pool", xs, dr["x1_s"][ti * 128:(ti + 1) * 128, :], xt[:, :], xd, dr["_d_x1"][ti])
        if os.environ.get('KDEBUG'):
            print('sbuf remaining', nc.sbuf_bytes_remaining)
        S.barrier()
        S.release_dma_sems()


def phase_ffn(nc, S, dr, SEQ, l, src, src_dep, dst, dst_dep):
    with ExitStack() as st:
        kb = KB(nc, S, st)
        C = load_consts(kb, dr)
        TS = 512
        NST = SEQ // TS
        NF = DFF // 128
        stage = Pool(kb, "stg", 3, [128, 704], F32, dma=True)
        wg, wgd = kb.tile("wg", [128, 8, DFF], BF16)
        wu, wud = kb.tile("wu", [128, 8, DFF], BF16)
        wd, wdd = kb.tile("wd", [128, NF, D], BF16)
        gf, gfd, gs = kb.tile("gffn", [128, D], F32, dma=True)
        kb.load("sp", gs, gf[:, :], dr[f"gffn{l}"][:, :], gfd)
        load_cast_weight(kb, stage, wg, wgd, lambda c, c0, w: dr[f"wg{l}"][:, c, c0:c0 + w], 8, DFF, engs=("dve", "pool", "act"))
        load_cast_weight(kb, stage, wu, wud, lambda c, c0, w: dr[f"wu{l}"][:, c, c0:c0 + w], 8, DFF, engs=("dve", "pool", "act"))
        load_cast_weight(kb, stage, wd, wdd, lambda c, c0, w: dr[f"wd{l}"][:, c, c0:c0 + w], NF, D, engs=("dve", "pool", "act"))
        xin = Pool(kb, "xin", 6, [128, D], F32, dma=True)
        hnp = Pool(kb, "hn", 2, [128, D], BF16)
        pools = {"junk": hnp.tiles[0][:2], "ss": Pool(kb, "ss", 3, [128, 4], F32), "hn": hnp,
                 "psT": Pool(kb, "psT", 1, [128, 1024], BF16, space="psum")}
        pools["junk"] = None
        hTp = Pool(kb, "hT", 1, [128, 8, TS], BF16)
        pg = Pool(kb, "pg", 2, [128, 512], F32, space="psum")
        pu = Pool(kb, "pu", 2, [128, 512], F32, space="psum")
        pO = Pool(kb, "pO", 2, [128, 512], F32, space="psum")
        sgp = Pool(kb, "sg", 2, [128, TS], F32)
        hidp = Pool(kb, "hid", 1, [128, NF, TS], BF16)
        for stile in range(NST):
            t0 = stile * TS
            hT, hTd, _ = hTp.next()
            xts = []
            for sub in range(TS // 128):
                ti = t0 // 128 + sub
                xt, xd, xs = xin.next()
                kb.load("sp", xs, xt[:, :], src[ti * 128:(ti + 1) * 128, :], xd, reads=[src_dep[ti]])
                norm_transpose(kb, C, xt, xd, gf, gfd, hT, hTd, sub * 128, pools)
                xts.append((xt, xd, xs))
            hid, hidd, _ = hidp.next()
            for f in range(NF):
                a, ad, _ = pg.next()
                kb.mm(a[:, 0:TS], ad, [(wg[:, k, f * 128:(f + 1) * 128], hT[:, k, :]) for k in range(8)], [wgd, hTd])
                b, bd, _ = pu.next()
                kb.mm(b[:, 0:TS], bd, [(wu[:, k, f * 128:(f + 1) * 128], hT[:, k, :]) for k in range(8)], [wud, hTd])
                sg, sgd, _ = sgp.next()
                kb.act(sg[:, :], a[:, 0:TS], AF.Silu, [ad], [sgd])
                kb.tt("dve", hid[:, f, :], sg[:, :], b[:, 0:TS], ALU.mult, [sgd, bd], [hidd])
            for sub in range(TS // 128):
                ti = t0 // 128 + sub
                xt, xd, xs = xts[sub]
                for half in range(2):
                    hs = slice(half * 512, (half + 1) * 512)
                    p, pd, _ = pO.next()
                    kb.mm(p[:, :], pd, [(hid[:, f, sub * 128:(sub + 1) * 128], wd[:, f, hs]) for f in range(NF)],
                          [hidd, wdd])
                    kb.tt("dve", xt[:, hs], xt[:, hs], p[:, :], ALU.add, [xd, pd], [xd])
                kb.store("pool", xs, dst[ti * 128:(ti + 1) * 128, :], xt[:, :], xd, dst_dep[ti])
        if os.environ.get('KDEBUG'):
            print('sbuf remaining', nc.sbuf_bytes_remaining)
        S.barrier()
        S.release_dma_sems()


def phase_c1(nc, S, dr, SEQ):
    with ExitStack() as st:
        kb = KB(nc, S, st)
        C = load_consts(kb, dr)
        TS = 512
        NST = SEQ // TS
        stage = Pool(kb, "stg", 4, [128, 1024], F32, dma=True)
        w_in, w_ind = kb.tile("w_in1", [128, 8, OIN], BF16)
        gmix, gmixd, gs = kb.tile("gmix1", [128, D], F32, dma=True)
        kb.load("sp", gs, gmix[:, :], dr["gmix1"][:, :], gmixd)
        load_cast_weight(kb, stage, w_in, w_ind, lambda c, c0, w: dr["w_in1"][:, c, c0:c0 + w], 8, OIN,
                         engs=("dve", "pool", "act"))
        xin = Pool(kb, "xin", 3, [128, D], F32, dma=True)
        pools = {"junk": kb.tile("junk", [128, D], BF16), "ss": Pool(kb, "ss", 3, [128, 4], F32),
                 "hn": Pool(kb, "hn", 2, [128, D], BF16),
                 "psT": Pool(kb, "psT", 1, [128, 1024], BF16, space="psum")}
        hTp = Pool(kb, "hT", 1, [128, 8, TS], BF16)
        pA = Pool(kb, "pA", 2, [128, 512], F32, space="psum")
        pB = Pool(kb, "pB", 2, [128, 512], F32, space="psum")
        pv = Pool(kb, "pv", 3, [128, 512], F32, space="psum")
        cosp = Pool(kb, "cos", 2, [128, TS], F32, dma=True)
        sinp = Pool(kb, "sin", 2, [128, TS], F32, dma=True)
        tp = [Pool(kb, f"t{i}", 2, [128, TS], F32) for i in range(4)]
        qkp = Pool(kb, "qk", 2, [128, 4, 8, 128], BF16, dma=True)
        vtp = Pool(kb, "vt", 2, [128, 2048], BF16, dma=True)
        sgp = Pool(kb, "sgt", 2, [128, 2048], BF16, dma=True)
        for stile in range(NST):
            t0 = stile * TS
            hT, hTd, _ = hTp.next()
            for sub in range(4):
                ti = t0 // 128 + sub
                xt, xd, xs = xin.next()
                kb.load("sp", xs, xt[:, :], dr["x2_s"][ti * 128:(ti + 1) * 128, :], xd, reads=[dr["_d_x2"][ti]])
                norm_transpose(kb, C, xt, xd, gmix, gmixd, hT, hTd, sub * 128, pools)
            cs, csd, css = cosp.next()
            kb.load("sp", css, cs[:, :], dr["cosR"][:, t0:t0 + TS], csd)
            sn, snd, sns = sinp.next()
            kb.load("sp", sns, sn[:, :], dr["sinR"][:, t0:t0 + TS], snd)
            for which, col, dst, ddep in ((0, 0, "qT_s", "_d_qT"), (1, 1024, "kT_s", "_d_kT")):
                qk, qkd, qks = qkp.next()
                for h in range(4):
                    a, ad, _ = pA.next()
                    c0 = col + h * 256
                    kb.mm(a[:, :], ad, [(w_in[:, kk, c0:c0 + 128], hT[:, kk, :]) for kk in range(8)], [w_ind, hTd])
                    b, bd, _ = pB.next()
                    kb.mm(b[:, :], bd, [(w_in[:, kk, c0 + 128:c0 + 256], hT[:, kk, :]) for kk in range(8)], [w_ind, hTd])
                    t = [p.next() for p in tp]
                    kb.tt("dve", t[0][0][:, :], a[:, :], cs[:, :], ALU.mult, [ad, csd], [t[0][1]])
                    kb.tt("dve", t[1][0][:, :], b[:, :], sn[:, :], ALU.mult, [bd, snd], [t[1][1]])
                    kb.tt("pool", qk[:, :, h * 2, :], t[0][0][:, :].rearrange("p (s t) -> p s t", s=4),
                          t[1][0][:, :].rearrange("p (s t) -> p s t", s=4), ALU.subtract, [t[0][1], t[1][1]], [qkd])
                    kb.tt("dve", t[2][0][:, :], b[:, :], cs[:, :], ALU.mult, [bd, csd], [t[2][1]])
                    kb.tt("dve", t[3][0][:, :], a[:, :], sn[:, :], ALU.mult, [ad, snd], [t[3][1]])
                    kb.tt("pool", qk[:, :, h * 2 + 1, :], t[2][0][:, :].rearrange("p (s t) -> p s t", s=4),
                          t[3][0][:, :].rearrange("p (s t) -> p s t", s=4), ALU.add, [t[2][1], t[3][1]], [qkd])
                kb.store("pool", qks, dr[dst][stile * 4:(stile + 1) * 4, :, :, :].rearrange("s p c t -> p s c t"),
                         qk[:, :, :, :], qkd, dr[ddep][stile])
            for sub in range(4):
                ti = t0 // 128 + sub
                vt, vtd, vts = vtp.next()
                sg, sgd, sgs = sgp.next()
                for n in range(4):
                    p, pd, _ = pv.next()
                    kb.mm(p[:, :], pd, [(hT[:, kk, sub * 128:(sub + 1) * 128], w_in[:, kk, 2048 + n * 512:2048 + (n + 1) * 512])
                                        for kk in range(8)], [w_ind, hTd])
                    kb.cp("dve" if n % 2 == 0 else "act", vt[:, n * 512:(n + 1) * 512], p[:, :], [pd], [vtd])
                for n in range(4):
                    p, pd, _ = pv.next()
                    kb.mm(p[:, :], pd, [(hT[:, kk, sub * 128:(sub + 1) * 128], w_in[:, kk, 4096 + n * 512:4096 + (n + 1) * 512])
                                        for kk in range(8)], [w_ind, hTd])
                    kb.act(sg[:, n * 512:(n + 1) * 512], p[:, :], AF.Silu, [pd], [sgd])
                kb.store("pool", vts, dr["v_s"][ti * 128:(ti + 1) * 128, :], vt[:, :], vtd, dr["_d_v"][ti])
                kb.store("pool", sgs, dr["sg_s"][ti * 128:(ti + 1) * 128, :], sg[:, :], sgd, dr["_d_sg"][ti])
        if os.environ.get('KDEBUG'):
            print('sbuf remaining', nc.sbuf_bytes_remaining)
        S.barrier()
        S.release_dma_sems()


def phase_c2(nc, S, dr, SEQ, gchunk):
    with ExitStack() as st:
        kb = KB(nc, S, st)
        C = load_consts(kb, dr)
        eps, ident = C["eps"], C["ident"]
        NT = SEQ // 128
        stage = Pool(kb, "stg", 5, [128, 1024], F32, dma=True)
        w_o, w_od = kb.tile("w_out1", [128, 16, D], BF16)
        load_cast_weight(kb, stage, w_o, w_od, lambda c, c0, w: dr["w_out1"][:, c, c0:c0 + w], 16, D)
        gret, gretd, gs = kb.tile("gret", [128, 2048], F32, dma=True)
        kb.load("sp", gs, gret[:, :], dr["gret"][:, :], gretd)
        maskT, maskTd, gs = kb.tile("maskT", [128, 4, 128], F32, dma=True)
        kb.load("sp", gs, maskT[:, :, :], dr["maskT"][:, :, :], maskTd)
        zeta8, zeta8d, gs = kb.tile("zeta8", [128, 1024], F32, dma=True)
        kb.load("sp", gs, zeta8[:, :], dr["zeta8"][:, :], zeta8d)
        xi8, xi8d, gs = kb.tile("xi8", [128, 8, 128], F32, dma=True)
        kb.load("sp", gs, xi8[:, :, :], dr["xi8"][:, :, :], xi8d)
        Sf, _ = kb.tile("Sf", [128, 4, 2, 512], F32)
        Sb, _ = kb.tile("Sb", [128, 4, 2, 512], BF16)
        Sfd = [Dep(f"Sf{h}") for h in range(4)]
        Sbd = [Dep(f"Sb{h}") for h in range(4)]
        qp = Pool(kb, "q", 3, [128, 8, 128], BF16, dma=True)
        kp = Pool(kb, "k", 3, [128, 8, 128], BF16, dma=True)
        vp = Pool(kb, "v", 3, [128, 2048], BF16, dma=True)
        sgp = Pool(kb, "sg", 3, [128, 2048], BF16, dma=True)
        xin = Pool(kb, "xin", 4, [128, D], F32, dma=True)
        psT = Pool(kb, "psT", 1, [128, 1024], BF16, space="psum")
        paT = Pool(kb, "paT", 1, [128, 512], F32, space="psum")
        pop = Pool(kb, "po", 4, [128, 512], F32, space="psum")
        pstp = Pool(kb, "pst", 1, [128, 2, 512], F32, space="psum")
        pOp = paT
        kzp = Pool(kb, "kz", 2, [128, 1024], BF16)
        qxp = Pool(kb, "qx", 2, [128, 8, 128], BF16)
        aTp = Pool(kb, "aTs", 2, [128, 4, 128], BF16)
        ssp = Pool(kb, "ss", 6, [128, 4], F32)
        onp = Pool(kb, "on", 4, [128, 512], F32)
        junkp = Pool(kb, "junk", 4, [128, 512], BF16)
        gtp = Pool(kb, "gated", 2, [128, 2048], BF16)
        gTp = Pool(kb, "gT", 2, [128, 16, 128], BF16)
        def front(i):
            F = {}
            q, qd, qs = qp.next()
            kb.load("sp", qs, q[:, :, :], dr["qT_s"][i, :, :, :], qd, reads=[dr["_d_qT"][i // 4]])
            k, kd, ks = kp.next()
            kb.load("sp", ks, k[:, :, :], dr["kT_s"][i, :, :, :], kd, reads=[dr["_d_kT"][i // 4]])
            v, vd, vs = vp.next()
            kb.load("sp", vs, v[:, :], dr["v_s"][i * 128:(i + 1) * 128, :], vd, reads=[dr["_d_v"][i]])
            sg, sgd, sgs = sgp.next()
            kb.load("sp", sgs, sg[:, :], dr["sg_s"][i * 128:(i + 1) * 128, :], sgd, reads=[dr["_d_sg"][i]])
            xt, xd, xs = xin.next()
            kb.load("sp", xs, xt[:, :], dr["x2_s"][i * 128:(i + 1) * 128, :], xd, reads=[dr["_d_x2"][i]])
            aT, aTd, _ = paT.next()
            for h in range(4):
                kb.mm(aT[:, h * 128:(h + 1) * 128], aTd,
                      [(k[:, h * 2 + dc, :], q[:, h * 2 + dc, :]) for dc in range(2)], [kd, qd])
            aTs, aTsd, _ = aTp.next()
            kb.tt("dve", aTs[:, :, :], aT[:, :].rearrange("p (h j) -> p h j", h=4), maskT[:, :, :], ALU.mult,
                  [aTd, maskTd], [aTsd])
            qx, qxd = None, None
            if i > 0:
                qx, qxd, _ = qxp.next()
                kb.tt("pool", qx[:, :, :], q[:, :, :], xi8[:, :, :], ALU.mult, [qd, xi8d], [qxd])
            kz, kzd = None, None
            if i < NT - 1:
                pT, pTd, _ = psT.next()
                for c in range(8):
                    kb.tr(pT[:, c * 128:(c + 1) * 128], pTd, k[:, c, :], ident[0][:, :], [kd, ident[1]], inc=(c == 7))
                kz, kzd, _ = kzp.next()
                kb.tt("dve", kz[:, :], pT[:, :], zeta8[:, :], ALU.mult, [pTd, zeta8d], [kzd])
            F.update(q=q, qd=qd, k=k, kd=kd, v=v, vd=vd, sg=sg, sgd=sgd, xt=xt, xd=xd, xs=xs, aTs=aTs, aTsd=aTsd,
                     qx=qx, qxd=qxd, kz=kz, kzd=kzd)
            return F

        def middle(i, F):
            v, vd, sg, sgd = F["v"], F["vd"], F["sg"], F["sgd"]
            aTs, aTsd, qx, qxd, kz, kzd = F["aTs"], F["aTsd"], F["qx"], F["qxd"], F["kz"], F["kzd"]
            gated, gatedd, _ = gtp.next()
            H = []
            for h in range(4):
                vh = v[:, h * 512:(h + 1) * 512]
                po, pod, _ = pop.next()
                if i == 0:
                    kb.mm1(po[:, :], pod, aTs[:, h, :], vh, True, True, [aTsd, vd])
                else:
                    kb.mm1(po[:, :], pod, aTs[:, h, :], vh, True, False, [aTsd, vd], inc=False)
                    kb.mm1(po[:, :], pod, qx[:, h * 2, :], Sb[:, h, 0, :], False, False, [qxd, Sbd[h]], inc=False)
                    kb.mm1(po[:, :], pod, qx[:, h * 2 + 1, :], Sb[:, h, 1, :], False, True, [qxd, Sbd[h]])
                ss, ssd, _ = ssp.next()
                H.append((po, pod, ss, ssd))
            for h in range(4):
                po, pod, ss, ssd = H[h]
                junk, junkd, _ = junkp.next()
                kb.act(junk[:, :], po[:, :], AF.Square, [pod], [junkd, ssd], accum_out=ss[:, 0:1])
            if i < NT - 1:
                for h in range(4):
                    vh = v[:, h * 512:(h + 1) * 512]
                    pst, pstd, _ = pstp.next()
                    for dc in range(2):
                        kb.mm1(pst[:, dc, :], pstd, kz[:, h * 256 + dc * 128:h * 256 + (dc + 1) * 128], vh, True, True,
                               [kzd, vd], inc=(dc == 1))
                    if i == 0:
                        kb.cp("dve", Sf[:, h, :, :], pst[:, :, :], [pstd], [Sfd[h]])
                    else:
                        kb.stt("dve", Sf[:, h, :, :], Sf[:, h, :, :], float(gchunk[h]), pst[:, :, :], ALU.mult, ALU.add,
                               [Sfd[h], pstd], [Sfd[h]])
            for h in range(4):
                po, pod, ss, ssd = H[h]
                kb.act(ss[:, 0:1], ss[:, 0:1], AF.Ln, [ssd, eps[1]], [ssd], scale=1.0 / 512, bias=eps[0][:, 0:1])
            for h in range(4):
                po, pod, ss, ssd = H[h]
                kb.act(ss[:, 0:1], ss[:, 0:1], AF.Exp, [ssd], [ssd], scale=-0.5)
            if i < NT - 1:
                for h in range(4):
                    kb.cp("act", Sb[:, h, :, :], Sf[:, h, :, :], [Sfd[h]], [Sbd[h]])
            ons = []
            for h in range(4):
                po, pod, ss, ssd = H[h]
                on, ond, _ = onp.next()
                kb.stt("dve", on[:, :], po[:, :], ss[:, 0:1], gret[:, h * 512:(h + 1) * 512], ALU.mult, ALU.mult,
                       [pod, ssd, gretd], [ond])
                ons.append((on, ond))
            for h in range(4):
                on, ond = ons[h]
                kb.tt("pool", gated[:, h * 512:(h + 1) * 512], on[:, :], sg[:, h * 512:(h + 1) * 512], ALU.mult,
                      [ond, sgd], [gatedd])
            F["gated"], F["gatedd"] = gated, gatedd

        def back(i, F):
            gated, gatedd, xt, xd, xs = F["gated"], F["gatedd"], F["xt"], F["xd"], F["xs"]
            gT, gTd, _ = gTp.next()
            for half in range(2):
                pT, pTd, _ = psT.next()
                for c in range(8):
                    cc = half * 8 + c
                    kb.tr(pT[:, c * 128:(c + 1) * 128], pTd, gated[:, cc * 128:(cc + 1) * 128], ident[0][:, :],
                          [gatedd, ident[1]], inc=(c == 7))
                kb.cp("act", gT[:, half * 8:(half + 1) * 8, :], pT[:, :].rearrange("p (c t) -> p c t", c=8), [pTd], [gTd])
            for half in range(2):
                hs = slice(half * 512, (half + 1) * 512)
                p, pd, _ = pOp.next()
                kb.mm(p[:, :], pd, [(gT[:, c, :], w_o[:, c, hs]) for c in range(16)], [gTd, w_od])
                kb.tt("dve", xt[:, hs], xt[:, hs], p[:, :], ALU.add, [xd, pd], [xd])
            kb.store("pool", xs, dr["x3_s"][i * 128:(i + 1) * 128, :], xt[:, :], xd, dr["_d_x3"][i])

        Fcur = front(0)
        for i in range(NT):
            middle(i, Fcur)
            Fnext = front(i + 1) if i + 1 < NT else None
            back(i, Fcur)
            Fcur = Fnext
        if os.environ.get('KDEBUG'):
            print('sbuf remaining', nc.sbuf_bytes_remaining)
        S.barrier()
        S.release_dma_sems()


class LazyDR(dict):
    def __init__(self, nc, specs):
        super().__init__()
        self.nc = nc
        self.specs = specs
        self.used = []

    def __missing__(self, name):
        shape, dt = self.specs[name]
        ap = self.nc.dram_tensor(name, list(shape), dt, kind="ExternalInput").ap()
        self[name] = ap
        self.used.append(name)
        return ap


def build_program(SEQ, debug=None, upto="all"):
    nc = bass.Bass("TRN2", target_bir_lowering=False)
    specs = {"x": ([SEQ, D], F32)}
    for nm, shp, dt in (("ident", [128, 128], BF16), ("ones", [128, 128], BF16), ("eps", [128, 1], F32),
                        ("ident32x2", [128, 64], F32), ("gmix0", [128, D], F32), ("gvn", [128, 512], F32), ("bsb", [128, 512], F32),
                        ("gqa", [128, 3], F32), ("gkva", [128, 2], F32),
                        ("w_in0", [128, 8, EIN], F32), ("wsT", [128, 4, 128], F32),
                        ("w_uq", [128, 3, 768], F32), ("w_ukv", [128, 2, 1024], F32),
                        ("w_oa", [128, 4, D], F32), ("w_ob", [128, 4, D], F32),
                        ("gq", [96, 1], F32), ("gk", [96, 1], F32), ("pfull", [96, 96], BF16),
                        ("cosM", [32, SEQ], F32), ("sinM", [32, SEQ], F32),
                        ("gffn0", [128, D], F32), ("wg0", [128, 8, DFF], F32), ("wu0", [128, 8, DFF], F32),
                        ("wd0", [128, DFF // 128, D], F32),
                        ("gffn1", [128, D], F32), ("wg1", [128, 8, DFF], F32), ("wu1", [128, 8, DFF], F32),
                        ("wd1", [128, DFF // 128, D], F32),
                        ("gmix1", [128, D], F32), ("w_in1", [128, 8, OIN], F32), ("w_out1", [128, 16, D], F32),
                        ("cosR", [128, SEQ], F32), ("sinR", [128, SEQ], F32), ("gret", [128, 2048], F32),
                        ("maskT", [128, 4, 128], F32), ("xi8", [128, 8, 128], F32), ("zeta8", [128, 1024], F32),
                        ):
        specs[nm] = (shp, dt)
    dr = LazyDR(nc, specs)
    kind = "ExternalOutput" if debug else "Internal"
    NST = SEQ // 512
    NT = SEQ // 128

    def scratch(name, shape, dt, ntiles):
        dr[name] = nc.dram_tensor(name, list(shape), dt, kind=kind).ap()
        dr["_d_" + name[:-2]] = [Dep(name + str(i)) for i in range(ntiles)]
    scratch("mixa_s", [4, 128, SEQ], BF16, NST)
    scratch("cqn_s", [3, 128, SEQ], BF16, NST)
    scratch("ckvn_s", [2, 128, SEQ], BF16, NST)
    scratch("kpe_s", [32, SEQ], F32, NST)
    scratch("x1_s", [SEQ, D], F32, NT)
    scratch("x2_s", [SEQ, D], F32, NT)
    scratch("x3_s", [SEQ, D], F32, NT)
    scratch("qT_s", [NT, 128, 8, 128], BF16, NST)
    scratch("kT_s", [NT, 128, 8, 128], BF16, NST)
    scratch("v_s", [SEQ, 2048], BF16, NT)
    scratch("sg_s", [SEQ, 2048], BF16, NT)
    dr["out"] = nc.dram_tensor("out", [SEQ, D], F32, kind="ExternalOutput").ap()
    out_dep = [Dep(f"out{i}") for i in range(NT)]
    order = ["a1", "a2", "b", "c1", "c2", "d"]
    last = order.index(upto) if upto in order else len(order) - 1
    with ExitStack() as st:
        S = Sched(nc, st)
        phase_a1(nc, S, dr, SEQ)
        if last >= 1:
            phase_a2(nc, S, dr, SEQ)
        if last >= 2:
            phase_ffn(nc, S, dr, SEQ, 0, dr["x1_s"], dr["_d_x1"], dr["x2_s"], dr["_d_x2"])
        if last >= 3:
            phase_c1(nc, S, dr, SEQ)
        if last >= 4:
            phase_c2(nc, S, dr, SEQ, ret_consts()[3])
        if last >= 5:
            phase_ffn(nc, S, dr, SEQ, 1, dr["x3_s"], dr["_d_x3"], dr["out"], out_dep)
        S.barrier()
        print("ops", S.n_ops, "waits", S.n_wait, "sems", S.nsem)
    nc._used_inputs = list(dr.used)
    return nc


def host_inputs(inp, b, SEQ):
    f = np.float32
    c = np.ascontiguousarray
    m = {}
    m["x"] = c(inp["x"][b])
    m["ident"] = np.eye(128, dtype=ml_dtypes.bfloat16)
    m["ones"] = np.ones((128, 128), dtype=ml_dtypes.bfloat16)
    m["ident32x2"] = np.concatenate([np.eye(64, dtype=f), np.eye(64, dtype=f)], 0)
    m["eps"] = np.full((128, 1), EPS, dtype=f)
    m["gmix0"] = c(np.broadcast_to(inp["norm_mix"][0][None, :], (128, D)))
    m["gvn"] = c(np.broadcast_to(inp["gm_v_norm"][0].reshape(1, 512), (128, 512)))
    m["bsb"] = c(np.broadcast_to(inp["gm_b_s"][0].reshape(1, 512), (128, 512)))
    m["gqa"] = c(inp["mla_q_a_norm"][0].reshape(3, 128).T)
    m["gkva"] = c(inp["mla_kv_a_norm"][0].reshape(2, 128).T)
    m["w_in0"] = c(inp["even_w_in"][0].reshape(8, 128, EIN).transpose(1, 0, 2))
    m["wsT"] = c(inp["gm_w_s"][0].transpose(2, 0, 1))
    m["w_uq"] = c(inp["mla_w_uq"][0].reshape(3, 128, 768).transpose(1, 0, 2))
    m["w_ukv"] = c(inp["mla_w_ukv"][0].reshape(2, 128, 1024).transpose(1, 0, 2))
    wo = inp["even_w_out"][0]
    m["w_oa"] = c(wo[0:512].reshape(4, 128, D).transpose(1, 0, 2))
    m["w_ob"] = c(wo[512:1024].reshape(4, 128, D).transpose(1, 0, 2))
    m["gq"] = c(inp["mla_q_norm"][0].reshape(96, 1))
    m["gk"] = c(inp["mla_k_norm"][0].reshape(96, 1))
    m.update(const_tables(SEQ))
    m["gmix1"] = c(np.broadcast_to(inp["norm_mix"][1][None, :], (128, D)))
    m["w_in1"] = c(inp["odd_w_in"][0].reshape(8, 128, OIN).transpose(1, 0, 2))
    m["w_out1"] = c(inp["odd_w_out"][0].reshape(16, 128, D).transpose(1, 0, 2))
    m["gret"] = c(np.broadcast_to(inp["ret_out_norm"][0].reshape(1, 2048), (128, 2048)))
    for l in range(2):
        m[f"gffn{l}"] = c(np.broadcast_to(inp["norm_ffn"][l][None, :], (128, D)))
        m[f"wg{l}"] = c(inp["ffn_w_gate"][l].reshape(8, 128, DFF).transpose(1, 0, 2))
        m[f"wu{l}"] = c(inp["ffn_w_up"][l].reshape(8, 128, DFF).transpose(1, 0, 2))
        m[f"wd{l}"] = c(inp["ffn_w_down"][l].reshape(DFF // 128, 128, D).transpose(1, 0, 2))
    return m


_CT = {}


def ret_consts():
    H, CH = 4, 128
    lg = np.log(1.0 - 2.0 ** (-5.0 - np.arange(H, dtype=np.float64)))
    j = np.arange(CH, dtype=np.float64)
    diff = j[None, :] - j[:, None]
    sc = 256.0 ** -0.5
    maskT = np.zeros((CH, H, CH), dtype=np.float64)
    for h in range(H):
        maskT[:, h, :] = np.where(diff >= 0, np.exp(lg[h] * np.maximum(diff, 0.0)), 0.0) * sc
    xi = np.exp(lg[:, None] * (j[None, :] + 1.0))
    xib = np.broadcast_to(xi[None, :, :], (128, H, CH))
    zeta = (np.exp(lg[None, :] * (CH - 1.0 - j[:, None])) * sc)
    gchunk = np.exp(lg * CH)
    return (np.ascontiguousarray(maskT.astype(np.float32)), np.ascontiguousarray(xib.astype(np.float32)),
            np.ascontiguousarray(zeta.astype(np.float32)), gchunk)


def const_tables(SEQ):
    if SEQ in _CT:
        return _CT[SEQ]
    f = np.float32
    m = {}
    pos = np.arange(SEQ, dtype=f)
    inv = (np.float32(10000.0) ** (-np.arange(16, dtype=f) / np.float32(16))).astype(f)
    ang = (pos[None, :] * inv[:, None]).astype(f)
    m["cosM"] = np.ascontiguousarray(np.concatenate([np.cos(ang), np.cos(ang)], 0).astype(f))
    m["sinM"] = np.ascontiguousarray(np.concatenate([np.sin(ang), np.sin(ang)], 0).astype(f))
    pfull = np.zeros((96, 96), dtype=f)
    for i in range(16):
        pfull[64 + i + 16, 64 + i] = -1.0
        pfull[64 + i, 64 + i + 16] = 1.0
    m["pfull"] = pfull.astype(ml_dtypes.bfloat16)
    invr = (np.float32(10000.0) ** (-np.arange(128, dtype=f) / np.float32(128))).astype(f)
    angr = (pos[None, :] * invr[:, None]).astype(f)
    m["cosR"] = np.ascontiguousarray(np.cos(angr).astype(f))
    m["sinR"] = np.ascontiguousarray(np.sin(angr).astype(f))
    maskT, xib, zeta, _ = ret_consts()
    m["maskT"] = maskT
    m["xi8"] = np.ascontiguousarray(np.repeat(xib, 2, axis=1))
    m["zeta8"] = np.ascontiguousarray(np.repeat(zeta, 256, axis=1))
    _CT[SEQ] = m
    return m


def kernel(**inputs):
    inp = {k: np.asarray(v) for k, v in inputs.items()}
    B, SEQ, _ = inp["x"].shape
    nc = build_program(SEQ)
    in_maps = [{k: v for k, v in host_inputs(inp, b, SEQ).items() if k in nc._used_inputs} for b in range(B)]
    res = run_bass_kernel_spmd(nc, in_maps, core_ids=list(range(B)))
    return np.stack([np.asarray(r["out"]) for r in res.results], axis=0).astype(np.float32)
```

```python
import math
from contextlib import ExitStack
import numpy as np
import ml_dtypes
import concourse.bass as bass
import concourse.mybir as mybir
from concourse.bass_utils import run_bass_kernel_spmd

F32 = mybir.dt.float32
BF16 = mybir.dt.bfloat16
AF = mybir.ActivationFunctionType
ALU = mybir.AluOpType

D = 1024
EIN = 1696
DFF = 2816
OIN = 6144
EPS = 1e-6
NCORES = 4


class Dep:
    __slots__ = ("w", "r", "name", "psum")

    def __init__(self, name="", psum=False):
        self.w = None
        self.r = []
        self.name = name
        self.psum = psum


class Sched:
    ENG = ("pe", "act", "dve", "pool", "sp")

    def __init__(self, nc, stack, sem_limit=28000):
        self.nc = nc
        self.stack = stack
        self.e = {"pe": nc.tensor, "act": nc.scalar, "dve": nc.vector,
                  "pool": nc.gpsimd, "sp": nc.sync}
        self.sem_limit = sem_limit
        self.sem = {}
        self.cnt = {}
        self.nsem = 0
        self.pending = {}
        for k in ("pe", "act", "dve", "pool"):
            self._new_sem(k)
        self.seen = {k: {} for k in self.ENG}
        self.n_wait = 0
        self.n_ops = 0
        self.all_tickets = {}
        self.free_dma = {}
        self.live_dma = []

    def _alloc_sem(self, name):
        self.nsem += 1
        return self.stack.enter_context(self.nc.semaphore(f"{name}_{self.nsem}"))

    def _new_sem(self, k):
        self.sem[k] = self._alloc_sem("s" + k)
        self.cnt[k] = 0

    def new_dma_sem(self, kind="hw"):
        fl = self.free_dma.setdefault(kind, [])
        s = fl.pop() if fl else [self._alloc_sem("sdma" + kind), 0]
        self.live_dma.append((kind, s))
        return s

    def release_dma_sems(self):
        for kind, s in self.live_dma:
            self.free_dma[kind].append(s)
        self.live_dma = []

    def _wait_for(self, e, reads, writes):
        need = {}

        def add(t):
            if t is None:
                return
            sem, val = t
            k = id(sem)
            if k not in need or need[k][1] < val:
                need[k] = (sem, val)
        for b in reads:
            add(b.w)
            if b.psum:
                for t in b.r:
                    if t[0] is not self.sem.get(e):
                        add(t)
        for b in writes:
            add(b.w)
            for t in b.r:
                add(t)
        eng = self.e[e]
        seen = self.seen[e]
        for k, (sem, val) in need.items():
            if e == "pe" and sem is self.sem.get("pe"):
                continue
            if seen.get(k, 0) < val:
                eng.wait_ge(sem, val)
                seen[k] = val
                self.n_wait += 1

    def _record(self, t, reads, writes):
        self.all_tickets[id(t[0])] = t if (id(t[0]) not in self.all_tickets or self.all_tickets[id(t[0])][1] < t[1]) else self.all_tickets[id(t[0])]
        for b in reads:
            b.r.append(t)
            if len(b.r) > 16:
                best = {}
                for sem, val in b.r:
                    k = id(sem)
                    if k not in best or best[k][1] < val:
                        best[k] = (sem, val)
                b.r = list(best.values())
        for b in writes:
            b.w = t
            b.r = []

    def op(self, e, fn, reads=(), writes=(), inc=True):
        self._wait_for(e, reads, writes)
        ins = fn(self.e[e])
        self.n_ops += 1
        if self.cnt[e] >= self.sem_limit and not self.pending.get(e):
            self._new_sem(e)
        self.pending[e] = not inc
        if inc:
            self.cnt[e] += 1
            ins.then_inc(self.sem[e], 1)
            t = (self.sem[e], self.cnt[e])
        else:
            t = (self.sem[e], self.cnt[e] + 1)
        self._record(t, reads, writes)
        return ins

    def dma(self, q, semslot, outs_ins, reads=(), writes=()):
        if semslot[1] >= self.sem_limit:
            semslot[0] = self._alloc_sem("sdma")
            semslot[1] = 0
        self._wait_for(q, reads, writes)
        eng = self.e[q]
        for (o, i) in outs_ins:
            eng.dma_start(out=o, in_=i).then_inc(semslot[0], 16)
            semslot[1] += 16
        t = (semslot[0], semslot[1])
        self._record(t, reads, writes)

    def barrier(self):
        for e in self.ENG:
            eng = self.e[e]
            seen = self.seen[e]
            for k, (sem, val) in list(self.all_tickets.items()):
                if seen.get(k, 0) < val:
                    eng.wait_ge(sem, val)
                    seen[k] = val


class Pool:
    def __init__(self, kb, name, n, shape, dtype, space="sbuf", dma=False):
        self.n = n
        self.i = 0
        self.tiles = []
        for j in range(n):
            if space == "sbuf":
                t = kb.st.enter_context(kb.nc.sbuf_tensor(f"{name}{j}_{kb.uid()}", list(shape), dtype))
            else:
                t = kb.st.enter_context(kb.nc.psum_tensor(f"{name}{j}_{kb.uid()}", list(shape), dtype))
            self.tiles.append((t, Dep(f"{name}{j}", psum=(space == "psum")), kb.S.new_dma_sem() if dma else None))

    def next(self):
        r = self.tiles[self.i % self.n]
        self.i += 1
        return r


_UID = [0]
_SSEM = {}


class KB:
    def __init__(self, nc, S, st):
        self.nc = nc
        self.S = S
        self.st = st
        self._uid = 0
        self.ssem = {}

    def uid(self):
        _UID[0] += 1
        return _UID[0]

    def tile(self, name, shape, dt, dma=False):
        t = self.st.enter_context(self.nc.sbuf_tensor(f"{name}_{self.uid()}", list(shape), dt))
        if dma:
            return t, Dep(name), self.S.new_dma_sem()
        return t, Dep(name)

    def sub(self, st):
        k = KB(self.nc, self.S, st)
        k._uid = self._uid + 1000
        return k

    def mm(self, out, od, pairs, reads):
        n = len(pairs)
        for i, (l, r) in enumerate(pairs):
            self.S.op("pe", lambda e, l=l, r=r, i=i: e.matmul(out, lhsT=l, rhs=r, start=(i == 0), stop=(i == n - 1)),
                      reads=reads, writes=[od], inc=(i == n - 1))

    def mm1(self, out, od, l, r, start, stop, reads, inc=True):
        self.S.op("pe", lambda e: e.matmul(out, lhsT=l, rhs=r, start=start, stop=stop),
                  reads=reads, writes=[od], inc=inc)

    def tr(self, out, od, in_, ident, reads, inc=True):
        self.S.op("pe", lambda e: e.transpose(out=out, in_=in_, identity=ident), reads=reads, writes=[od], inc=inc)

    def act(self, out, in_, func, reads, writes, **kw):
        self.S.op("act", lambda e: e.activation(out=out, in_=in_, func=func, **kw), reads=reads, writes=writes)

    def tt(self, eng, out, in0, in1, op, reads, writes):
        self.S.op(eng, lambda e: e.tensor_tensor(out=out, in0=in0, in1=in1, op=op), reads=reads, writes=writes)

    def ts(self, eng, out, in0, s1, s2, op0, op1, reads, writes):
        if s2 is None:
            self.S.op(eng, lambda e: e.tensor_scalar(out=out, in0=in0, scalar1=s1, scalar2=None, op0=op0), reads=reads, writes=writes)
        else:
            self.S.op(eng, lambda e: e.tensor_scalar(out=out, in0=in0, scalar1=s1, scalar2=s2, op0=op0, op1=op1), reads=reads, writes=writes)

    def stt(self, eng, out, in0, scalar, in1, op0, op1, reads, writes):
        self.S.op(eng, lambda e: e.scalar_tensor_tensor(out=out, in0=in0, scalar=scalar, in1=in1, op0=op0, op1=op1),
                  reads=reads, writes=writes)

    def cp(self, eng, out, in_, reads, writes):
        if eng == "act":
            self.S.op("act", lambda e: e.copy(out=out, in_=in_), reads=reads, writes=writes)
        else:
            self.S.op(eng, lambda e: e.tensor_copy(out=out, in_=in_), reads=reads, writes=writes)

    def recip(self, out, in_, reads, writes):
        self.S.op("dve", lambda e: e.reciprocal(out=out, in_=in_), reads=reads, writes=writes)

    def memset(self, eng, ap, val, writes):
        self.S.op(eng, lambda e: e.memset(ap, val), reads=[], writes=writes)

    def rstd(self, out, in_, scale, epst, reads, writes):
        self.act(out, in_, AF.Ln, reads, writes, scale=scale, bias=epst)
        self.act(out, out, AF.Exp, writes, writes, scale=-0.5)

    def load(self, q, sem, out, in_, od, reads=()):
        self.S.dma(q, sem, [(out, in_)], reads=list(reads), writes=[od])

    def store(self, q, sem, out, in_, src_dep, dram_dep):
        k = id(sem)
        if k not in self.ssem:
            self.ssem[k] = (sem, self.S.new_dma_sem("sw"))
        self.S.dma(q, self.ssem[k][1], [(out, in_)], reads=[src_dep], writes=[dram_dep])


def load_cast_weight(kb, stage_pool, dst, dst_dep, src_ap_fn, nchunks, ncols, engs=("dve", "pool"), p0=0, np_=None):
    P = dst.shape[0] if np_ is None else np_
    CW = stage_pool.tiles[0][0].shape[1]
    i = 0
    for c in range(nchunks):
        for c0 in range(0, ncols, CW):
            w = min(CW, ncols - c0)
            stg, sd, ssem = stage_pool.next()
            kb.load("sp", ssem, stg[p0:p0 + P, 0:w], src_ap_fn(c, c0, w), sd)
            kb.cp(engs[i % len(engs)], dst[p0:p0 + P, c, c0:c0 + w], stg[p0:p0 + P, 0:w], [sd], [dst_dep])
            i += 1


def norm_transpose(kb, C, xt, xd, gb, gbd, hT, hTd, col0, pools):
    ss, ssd = pools["ss"].next()[:2]
    hn, hnd = pools["hn"].next()[:2]
    junk, junkd = pools["junk"] if pools.get("junk") is not None else (hn, hnd)
    kb.act(junk[:, :], xt[:, :], AF.Square, [xd], [junkd, ssd], accum_out=ss[:, 0:1])
    kb.rstd(ss[:, 0:1], ss[:, 0:1], 1.0 / D, C["eps"][0][:, 0:1], [ssd, C["eps"][1]], [ssd])
    kb.stt("dve", hn[:, :], xt[:, :], ss[:, 0:1], gb[:, :], ALU.mult, ALU.mult, [xd, ssd, gbd], [hnd])
    pT, pTd = pools["psT"].next()[:2]
    for c in range(8):
        kb.tr(pT[:, c * 128:(c + 1) * 128], pTd, hn[:, c * 128:(c + 1) * 128], C["ident"][0][:, :],
              [hnd, C["ident"][1]], inc=(c == 7))
    kb.cp("act", hT[:, :, col0:col0 + 128], pT[:, :].rearrange("p (c t) -> p c t", c=8), [pTd], [hTd])


def load_consts(kb, dr):
    C = {}
    for name, shape, dt in (("ident", [128, 128], BF16), ("ones", [128, 128], BF16),
                            ("eps", [128, 1], F32)):
        t, d, s = kb.tile(name, shape, dt, dma=True)
        kb.load("sp", s, t[:, :], dr[name][:, :], d)
        C[name] = (t, d)
    return C


import os
STOP = int(os.environ.get('KSTOP', '99'))


def phase_a1(nc, S, dr, SEQ):
    with ExitStack() as st:
        kb = KB(nc, S, st)
        C = load_consts(kb, dr)
        TS = 512
        NST = SEQ // TS
        w_in, w_ind = kb.tile("w_in", [128, 8, EIN], BF16)
        wsT, wsTd = kb.tile("wsT", [128, 4, 128], BF16)
        stage = Pool(kb, "stg", 5, [128, 1024], F32, dma=True)
        gmix, gmixd, gs = kb.tile("gmix", [128, D], F32, dma=True)
        kb.load("sp", gs, gmix[:, :], dr["gmix0"][:, :], gmixd)
        gvn, gvnd, gs = kb.tile("gvn", [128, 512], F32, dma=True)
        kb.load("sp", gs, gvn[:, :], dr["gvn"][:, :], gvnd)
        bsb, bsbd, gs = kb.tile("bsb", [128, 512], F32, dma=True)
        kb.load("sp", gs, bsb[:, :], dr["bsb"][:, :], bsbd)
        gqa, gqad, gs = kb.tile("gqa", [128, 3], F32, dma=True)
        kb.load("sp", gs, gqa[:, :], dr["gqa"][:, :], gqad)
        gkva, gkvad, gs = kb.tile("gkva", [128, 2], F32, dma=True)
        kb.load("sp", gs, gkva[:, :], dr["gkva"][:, :], gkvad)
        load_cast_weight(kb, stage, w_in, w_ind, lambda c, c0, w: dr["w_in0"][:, c, c0:c0 + w], 8, EIN)
        load_cast_weight(kb, stage, wsT, wsTd, lambda c, c0, w: dr["wsT"][:, c, c0:c0 + w], 4, 128)
        for g in range(4):
            kb.memset("pool", wsT[64:128, g, 0:64], 0.0, [wsTd])

        if STOP <= 0:
            S.barrier(); S.release_dma_sems(); return
        xin = Pool(kb, "xin", 3, [128, D], F32, dma=True)
        junk = kb.tile("junk", [128, D], BF16)
        pools = {"junk": junk, "ss": Pool(kb, "ss", 3, [128, 4], F32), "hn": Pool(kb, "hn", 2, [128, D], BF16),
                 "psT": Pool(kb, "psT", 2, [128, 1024], BF16, space="psum")}
        ps = Pool(kb, "ps", 4, [128, 512], F32, space="psum")
        hTp = Pool(kb, "hT", 2, [128, 8, TS], BF16)
        uTp = Pool(kb, "uT", 1, [128, 4, TS], F32)
        cfp = Pool(kb, "cf", 2, [128, 3, TS], F32)
        sqp = Pool(kb, "sq", 2, [128, 3, TS], BF16)
        rsp = Pool(kb, "rs", 2, [128, TS], F32)
        cnp = Pool(kb, "cn", 2, [128, 3, TS], BF16, dma=True)
        kpp = Pool(kb, "kp", 2, [96, TS], F32, dma=True)
        vp = Pool(kb, "v", 2, [128, 512], F32)
        vnp = Pool(kb, "vn", 2, [128, 512], BF16)
        tmpp = Pool(kb, "tmp", 2, [128, 512], F32)
        mixp = Pool(kb, "mixa", 2, [128, 4, TS], BF16, dma=True)
        eps = C["eps"]
        ones = C["ones"]

        for stile in range(NST):
            t0 = stile * TS
            hT, hTd, _ = hTp.next()
            for sub in range(4):
                xt, xd, xs = xin.next()
                kb.load("sp", xs, xt[:, :], dr["x"][t0 + sub * 128:t0 + (sub + 1) * 128, :], xd)
                norm_transpose(kb, C, xt, xd, gmix, gmixd, hT, hTd, sub * 128, pools)
            if STOP <= 1:
                S.barrier(); S.release_dma_sems(); return
            uT, uTd, _ = uTp.next()
            for g in range(4):
                p, pd, _ = ps.next()
                kb.mm(p[:, :], pd, [(w_in[:, k, g * 128:(g + 1) * 128], hT[:, k, :]) for k in range(8)], [w_ind, hTd])
                kb.act(uT[:, g, :], p[:, :], AF.Gelu_apprx_tanh, [pd], [uTd])
            if STOP <= 2:
                S.barrier(); S.release_dma_sems(); return
            mixa, mixad, mixs = mixp.next()
            for sub in range(4):
                p, pd, _ = ps.next()
                kb.mm(p[:, :], pd, [(hT[:, k, sub * 128:(sub + 1) * 128], w_in[:, k, 512:1024]) for k in range(8)],
                      [w_ind, hTd])
                v, vd, _ = vp.next()
                kb.act(v[:, :], p[:, :], AF.Gelu_apprx_tanh, [pd], [vd])
                ss, ssd, _ = pools["ss"].next()
                jk, jkd = junk
                for g in range(4):
                    kb.act(jk[:, 0:128], v[:, g * 128:(g + 1) * 128], AF.Square, [vd], [jkd, ssd],
                           accum_out=ss[:, g:g + 1])
                kb.rstd(ss[:, 0:4], ss[:, 0:4], 1.0 / 128, eps[0][:, 0:1], [ssd, eps[1]], [ssd])
                vn, vnd, _ = vnp.next()
                for g in range(4):
                    kb.stt("dve", vn[:, g * 128:(g + 1) * 128], v[:, g * 128:(g + 1) * 128], ss[:, g:g + 1],
                           gvn[:, g * 128:(g + 1) * 128], ALU.mult, ALU.mult, [vd, ssd, gvnd], [vnd])
                p2, p2d, _ = ps.next()
                for g in range(4):
                    kb.mm(p2[:, g * 128:(g + 1) * 128], p2d, [(vn[:, g * 128:(g + 1) * 128], wsT[:, g, :])],
                          [vnd, wsTd])
                tmp, tmpd, _ = tmpp.next()
                kb.tt("dve", tmp[:, :], p2[:, :], bsb[:, :], ALU.add, [p2d, bsbd], [tmpd])
                kb.tt("pool", mixa[:, :, sub * 128:(sub + 1) * 128], tmp[:, :].rearrange("p (g t) -> p g t", g=4),
                      uT[:, :, sub * 128:(sub + 1) * 128], ALU.mult, [tmpd, uTd], [mixad])
            for g in range(4):
                kb.store("pool", mixs, dr["mixa_s"][g, :, t0:t0 + TS], mixa[:, g, :], mixad, dr["_d_mixa"][stile])
            if STOP <= 3:
                S.barrier(); S.release_dma_sems(); return
            for (nch, col, gt, gtd, dst, ddep, nfeat) in ((3, 1024, gqa, gqad, "cqn_s", "_d_cqn", 384),
                                                          (2, 1408, gkva, gkvad, "ckvn_s", "_d_ckvn", 256)):
                cf, cfd, _ = cfp.next()
                sq, sqd, _ = sqp.next()
                for c in range(nch):
                    p, pd, _ = ps.next()
                    kb.mm(p[:, :], pd, [(w_in[:, k, col + c * 128:col + (c + 1) * 128], hT[:, k, :]) for k in range(8)],
                          [w_ind, hTd])
                    kb.cp("dve", cf[:, c, :], p[:, :], [pd], [cfd])
                    kb.act(sq[:, c, :], p[:, :], AF.Square, [pd], [sqd])
                p, pd, _ = ps.next()
                kb.mm(p[:, :], pd, [(ones[0][:, :], sq[:, c, :]) for c in range(nch)], [ones[1], sqd])
                rs, rsd, _ = rsp.next()
                kb.rstd(rs[:, :], p[:, :], 1.0 / nfeat, eps[0][:, 0:1], [pd, eps[1]], [rsd])
                cn, cnd, cns = cnp.next()
                for c in range(nch):
                    kb.stt("dve", cn[:, c, :], cf[:, c, :], gt[:, c:c + 1], rs[:, :], ALU.mult, ALU.mult,
                           [cfd, gtd, rsd], [cnd])
                for c in range(nch):
                    if os.environ.get("KSKIP") == "cst":
                        continue
                    kb.store("pool", cns, dr[dst][c, :, t0:t0 + TS], cn[:, c, :], cnd, dr[ddep][stile])
            if STOP <= 4:
                S.barrier(); S.release_dma_sems(); return
            p, pd, _ = ps.next()
            kb.mm(p[64:96, :], pd, [(w_in[:, k, 1664:1696], hT[:, k, :]) for k in range(8)], [w_ind, hTd])
            kp, kpd, kps = kpp.next()
            kb.cp("dve", kp[64:96, :], p[64:96, :], [pd], [kpd])
            kb.store("pool", kps, dr["kpe_s"][:, t0:t0 + TS], kp[64:96, :], kpd, dr["_d_kpe"][stile])
        if os.environ.get('KDEBUG'):
            print('sbuf remaining', nc.sbuf_bytes_remaining)
        S.barrier()
        S.release_dma_sems()


def phase_a2(nc, S, dr, SEQ):
    with ExitStack() as st:
        kb = KB(nc, S, st)
        C = load_consts(kb, dr)
        ones, eps = C["ones"], C["eps"]
        TS = 256
        NST = SEQ // TS
        NT = SEQ // 128
        SCALE = 96.0 ** -0.5
        stage = Pool(kb, "stg", 2, [128, 768], F32, dma=True)
        w_uq, w_uqd = kb.tile("w_uq", [128, 3, 768], BF16)
        w_ukv, w_ukvd = kb.tile("w_ukv", [128, 2, 1024], BF16)
        w_oa, w_oad = kb.tile("w_oa", [128, 4, D], BF16)
        w_ob, w_obd = kb.tile("w_ob", [128, 4, D], BF16)
        load_cast_weight(kb, stage, w_uq, w_uqd, lambda c, c0, w: dr["w_uq"][:, c, c0:c0 + w], 3, 768)
        load_cast_weight(kb, stage, w_ukv, w_ukvd, lambda c, c0, w: dr["w_ukv"][:, c, c0:c0 + w], 2, 1024)
        load_cast_weight(kb, stage, w_oa, w_oad, lambda c, c0, w: dr["w_oa"][:, c, c0:c0 + w], 4, D)
        load_cast_weight(kb, stage, w_ob, w_obd, lambda c, c0, w: dr["w_ob"][:, c, c0:c0 + w], 4, D)
        gq, gqd, gs = kb.tile("gq", [96, 1], F32, dma=True)
        kb.load("sp", gs, gq[:, :], dr["gq"][:, :], gqd)
        gk, gkd, gs = kb.tile("gk", [96, 1], F32, dma=True)
        kb.load("sp", gs, gk[:, :], dr["gk"][:, :], gkd)
        pf, pfd, gs = kb.tile("pfull", [96, 96], BF16, dma=True)
        kb.load("sp", gs, pf[:, :], dr["pfull"][:, :], pfd)
        Kc = [kb.tile(f"Kc{h}", [96, SEQ], BF16)[0] for h in range(8)]
        Kdep = [Dep(f"K{i}") for i in range(NST)]
        Vc, _ = kb.tile("Vc", [128, NT, 4, 192], BF16)
        Vdep = [Dep(f"V{i}") for i in range(NT)]
        Vones = Dep("Vones")
        for pr in range(4):
            kb.memset("pool", Vc[:, :, pr, 64:128], 1.0, [Vones])
        id32, id32d, gs = kb.tile("id32", [128, 64], F32, dma=True)
        kb.load("sp", gs, id32[:, :], dr["ident32x2"][:, :], id32d)

        def vlhsT(kt, h):
            return Vc[:, kt, h // 2, 0:128] if h % 2 == 0 else Vc[:, kt, h // 2, 64:192]

        cqp = Pool(kb, "cq", 2, [128, 3, TS], BF16, dma=True)
        ckp = Pool(kb, "ck", 2, [128, 2, TS], BF16, dma=True)
        kpp = Pool(kb, "kp", 2, [96, TS], F32, dma=True)
        cosp = Pool(kb, "cos", 2, [96, TS], F32, dma=True)
        sinp = Pool(kb, "sin", 2, [96, TS], F32, dma=True)
        mixp = Pool(kb, "mixa", 2, [128, 4, TS], BF16, dma=True)
        xin = Pool(kb, "xin", 3, [128, D], F32, dma=True)
        psK = Pool(kb, "psK", 2, [128, 512], F32, space="psum")
        psQ = Pool(kb, "psQ", 2, [128, 512], F32, space="psum")
        pSp = Pool(kb, "pS", 3, [128, 512], F32, space="psum")
        pop = Pool(kb, "po", 1, [128, 512], F32, space="psum")
        sqpe, sqped = kb.tile("sqpe", [96, TS], BF16)
        sspe, ssped = kb.tile("sspe", [96, TS], F32)
        kpg, kpgd = kb.tile("kpg", [96, TS], F32)
        kpgb, kpgbd = kb.tile("kpgb", [96, TS], BF16)
        kper, kperd = kb.tile("kper", [96, TS], F32)
        t1p = Pool(kb, "t1", 2, [96, TS], F32)
        t1K = Pool(kb, "t1K", 1, [96, TS], F32)
        t2K = Pool(kb, "t2K", 1, [96, TS], F32)
        t2p = Pool(kb, "t2", 2, [96, TS], F32)
        sqK = Pool(kb, "sqK", 2, [96, TS], BF16)
        sqQ = Pool(kb, "sqQ", 2, [96, TS], BF16)
        rkK = Pool(kb, "rkK", 2, [96, TS], F32)
        rkQ = Pool(kb, "rkQ", 2, [96, TS], F32)
        qnp = Pool(kb, "qn", 2, [96, TS], F32)
        qnbp = Pool(kb, "qnb", 2, [96, TS], BF16)
        Qhp = Pool(kb, "Qh", 16, [96, TS], BF16)
        PTp = Pool(kb, "PT", 4, [128, TS], BF16)
        recp = Pool(kb, "rec", 2, [128, TS], F32)
        recsp = Pool(kb, "recs", 2, [128, TS], F32)
        obp = Pool(kb, "ob", 2, [128, 4, TS], BF16)

        PREP = {}

        def rr(*gens):
            gens = list(gens)
            while gens:
                for g_ in list(gens):
                    try:
                        next(g_)
                        yield
                    except StopIteration:
                        gens.remove(g_)

        def prep(stile):
            L = {}
            yield from prep_loads(stile, L)
            yield from rr(prep_k(stile, L), prep_vq(stile, L))

        def prep_loads(stile, L):
            t0 = stile * TS
            cq, cqd, cqs = cqp.next()
            kb.load("sp", cqs, cq[:, :, :], dr["cqn_s"][:, :, t0:t0 + TS].rearrange("c p t -> p c t"), cqd,
                    reads=[dr["_d_cqn"][t0 // 512]])
            ck, ckd, cks = ckp.next()
            kb.load("sp", cks, ck[:, :, :], dr["ckvn_s"][:, :, t0:t0 + TS].rearrange("c p t -> p c t"), ckd,
                    reads=[dr["_d_ckvn"][t0 // 512]])
            kp, kpd, kps = kpp.next()
            kb.load("sp", kps, kp[64:96, :], dr["kpe_s"][:, t0:t0 + TS], kpd, reads=[dr["_d_kpe"][t0 // 512]])
            cs, csd, css = cosp.next()
            kb.load("sp", css, cs[64:96, :], dr["cosM"][:, t0:t0 + TS], csd)
            sn, snd, sns = sinp.next()
            kb.load("sp", sns, sn[64:96, :], dr["sinM"][:, t0:t0 + TS], snd)
            L.update(cq=cq, cqd=cqd, ck=ck, ckd=ckd, kp=kp, kpd=kpd, cs=cs, csd=csd, sn=sn, snd=snd)
            yield

        def prep_k(stile, L):
            t0 = stile * TS
            ck, ckd, kp, kpd, cs, csd, sn, snd = (L[k_] for k_ in ("ck", "ckd", "kp", "kpd", "cs", "csd", "sn", "snd"))
            kb.act(sqpe[64:96, :], kp[64:96, :], AF.Square, [kpd], [sqped])
            kb.ts("dve", kpg[64:96, :], kp[64:96, :], gk[64:96, 0:1], None, ALU.mult, None, [kpd, gkd], [kpgd])
            yield
            kb.cp("pool", kpgb[64:96, :], kpg[64:96, :], [kpgd], [kpgbd])
            yield
            p, pd, _ = psK.next()
            kb.mm(p[0:96, 0:TS], pd, [(pf[64:96, 0:96], kpgb[64:96, :])], [pfd, kpgbd])
            yield
            t1, t1d, _ = t1K.next()
            kb.tt("dve", t1[64:96, :], p[64:96, 0:TS], sn[64:96, :], ALU.mult, [pd, snd], [t1d])
            t2, t2d, _ = t2K.next()
            kb.tt("pool", t2[64:96, :], kpg[64:96, :], cs[64:96, :], ALU.mult, [kpgd, csd], [t2d])
            yield
            kb.tt("pool", kper[64:96, :], t1[64:96, :], t2[64:96, :], ALU.add, [t1d, t2d], [kperd])
            p, pd, _ = psK.next()
            kb.mm(p[0:96, 0:TS], pd, [(ones[0][64:96, 0:96], sqpe[64:96, :])], [ones[1], sqped])
            yield
            kb.cp("dve", sspe[:, :], p[0:96, 0:TS], [pd], [ssped])
            yield
            for h in range(8):
                p, pd, _ = psK.next()
                kb.mm(p[0:64, 0:TS], pd, [(w_ukv[:, c, h * 128:h * 128 + 64], ck[:, c, :]) for c in range(2)],
                      [w_ukvd, ckd])
                yield
                sq, sqd, _ = sqK.next()
                kb.act(sq[0:64, :], p[0:64, 0:TS], AF.Square, [pd], [sqd])
                yield
                p2, p2d, _ = psK.next()
                kb.mm(p2[0:96, 0:TS], p2d, [(ones[0][0:64, 0:96], sq[0:64, :])], [ones[1], sqd])
                yield
                rk, rkd, _ = rkK.next()
                kb.tt("dve", rk[:, :], p2[0:96, 0:TS], sspe[:, :], ALU.add, [p2d, ssped], [rkd])
                yield
                kb.act(rk[:, :], rk[:, :], AF.Ln, [rkd, eps[1]], [rkd], scale=1.0 / 96, bias=eps[0][0:96, 0:1])
                yield
                kb.act(rk[:, :], rk[:, :], AF.Exp, [rkd], [rkd], scale=-0.5)
                yield
                kb.stt("dve", Kc[h][0:64, t0:t0 + TS], p[0:64, 0:TS], gk[0:64, 0:1], rk[0:64, :], ALU.mult, ALU.mult,
                       [pd, gkd, rkd], [Kdep[stile]])
                kb.tt("pool", Kc[h][64:96, t0:t0 + TS], kper[64:96, :], rk[64:96, :], ALU.mult,
                      [kperd, rkd], [Kdep[stile]])
                yield

        def prep_vq(stile, L):
            t0 = stile * TS
            cq, cqd, ck, ckd, cs, csd, sn, snd = (L[k_] for k_ in ("cq", "cqd", "ck", "ckd", "cs", "csd", "sn", "snd"))
            for sub in range(TS // 128):
                ti = t0 // 128 + sub
                p, pd, _ = psQ.next()
                kb.mm(p[:, :].rearrange("p (h e) -> p h e", h=8), pd,
                      [(ck[:, c, sub * 128:(sub + 1) * 128],
                        w_ukv[:, c, :].rearrange("p (h e) -> p h e", h=8)[:, :, 64:128]) for c in range(2)],
                      [w_ukvd, ckd])
                yield
                pv4 = p[:, :].rearrange("p (q two e) -> p q two e", q=4, two=2)
                kb.cp("act", Vc[:, ti, :, 0:64], pv4[:, :, 0, :], [pd], [Vdep[ti]])
                yield
                kb.cp("act", Vc[:, ti, :, 128:192], pv4[:, :, 1, :], [pd], [Vdep[ti]])
                yield
            Qs = []
            for h in range(8):
                p, pd, _ = psQ.next()
                kb.mm(p[0:96, 0:TS], pd, [(w_uq[:, c, h * 96:(h + 1) * 96], cq[:, c, :]) for c in range(3)],
                      [w_uqd, cqd])
                yield
                sq, sqd, _ = sqQ.next()
                kb.act(sq[0:96, :], p[0:96, 0:TS], AF.Square, [pd], [sqd])
                yield
                p2, p2d, _ = psQ.next()
                kb.mm(p2[0:96, 0:TS], p2d, [(ones[0][0:96, 0:96], sq[0:96, :])], [ones[1], sqd])
                yield
                rq, rqd, _ = rkQ.next()
                kb.act(rq[:, :], p2[0:96, 0:TS], AF.Ln, [p2d, eps[1]], [rqd], scale=1.0 / 96, bias=eps[0][0:96, 0:1])
                yield
                kb.act(rq[:, :], rq[:, :], AF.Exp, [rqd], [rqd], scale=-0.5)
                yield
                qn, qnd, _ = qnp.next()
                kb.stt("dve", qn[:, :], p[0:96, 0:TS], gq[:, 0:1], rq[:, :], ALU.mult, ALU.mult, [pd, gqd, rqd], [qnd])
                yield
                qnb, qnbd, _ = qnbp.next()
                kb.cp("pool", qnb[64:96, :], qn[64:96, :], [qnd], [qnbd])
                yield
                p3, p3d, _ = psQ.next()
                kb.mm(p3[0:96, 0:TS], p3d, [(pf[64:96, 0:96], qnb[64:96, :])], [pfd, qnbd])
                Qh, Qhd, _ = Qhp.next()
                kb.cp("pool", Qh[0:64, :], qn[0:64, :], [qnd], [Qhd])
                yield
                t1, t1d, _ = t1p.next()
                kb.tt("dve", t1[64:96, :], p3[64:96, 0:TS], sn[64:96, :], ALU.mult, [p3d, snd], [t1d])
                t2, t2d, _ = t2p.next()
                kb.tt("pool", t2[64:96, :], qn[64:96, :], cs[64:96, :], ALU.mult, [qnd, csd], [t2d])
                yield
                kb.tt("pool", Qh[64:96, :], t1[64:96, :], t2[64:96, :], ALU.add, [t1d, t2d], [Qhd])
                Qs.append((Qh, Qhd))
                yield
            PREP[stile] = Qs

        NPREP = 175
        LOOK = 2
        for _ in prep(0):
            pass
        for stile in range(NST):
            t0 = stile * TS
            nq = TS // 128
            nkt = nq * (stile + 1)
            g = prep(stile + 1) if stile + 1 < NST else None
            spi = -(-NPREP // (8 * nkt))
            Qs = PREP.pop(stile)
            ob, obd, _ = obp.next()
            for h in range(8):
                Qh, Qhd = Qs[h]
                po, pod, _ = pop.next()

                def issue_qk(kt):
                    j = kt - nq * stile
                    c0 = 128 * j if j > 0 else 0
                    pS, pSd, _ = pSp.next()
                    kb.mm(pS[:, c0:TS], pSd, [(Kc[h][0:96, kt * 128:(kt + 1) * 128], Qh[0:96, c0:TS])],
                          [Kdep[(kt * 128) // TS], Qhd])
                    return pS, pSd, j, c0
                pend = [issue_qk(kt) for kt in range(min(LOOK, nkt))]
                for kt in range(nkt):
                    if kt + LOOK < nkt:
                        pend.append(issue_qk(kt + LOOK))
                    pS, pSd, j, c0 = pend.pop(0)
                    PT, PTd, _ = PTp.next()
                    kb.act(PT[:, c0:TS], pS[:, c0:TS], AF.Exp, [pSd], [PTd], scale=SCALE)
                    if j >= 0:
                        kb.memset("pool", PT[64:128, c0:c0 + 64], 0.0, [PTd])
                    first, last = (kt == 0), (kt == nkt - 1)
                    kb.mm1(po[:, c0:TS], pod, vlhsT(kt, h), PT[:, c0:TS], first, last,
                           [Vdep[kt], Vones, PTd], inc=True)
                    if g is not None:
                        for _ in range(spi):
                            next(g, None)
                o_sl, d_sl = (slice(0, 64), slice(64, 128)) if h % 2 == 0 else (slice(64, 128), slice(0, 64))
                rec, recd, _ = recp.next()
                kb.recip(rec[d_sl, :], po[d_sl, 0:TS], [pod], [recd])
                psh, pshd, _ = pSp.next()
                kb.mm(psh[o_sl, 0:TS], pshd, [(id32[d_sl, 0:64], rec[d_sl, :])], [id32d, recd])
                recs, recsd, _ = recsp.next()
                kb.cp("act", recs[o_sl, :], psh[o_sl, 0:TS], [pshd], [recsd])
                kb.tt("dve", ob[o_sl, h // 2, :], po[o_sl, 0:TS], recs[o_sl, :], ALU.mult, [pod, recsd], [obd])
            if g is not None:
                for _ in g:
                    pass
            mixa, mixad, mixs = mixp.next()
            kb.load("sp", mixs, mixa[:, :, :], dr["mixa_s"][:, :, t0:t0 + TS].rearrange("g p t -> p g t"), mixad,
                    reads=[dr["_d_mixa"][t0 // 512]])
            for sub in range(TS // 128):
                ti = t0 // 128 + sub
                xt, xd, xs = xin.next()
                kb.load("sp", xs, xt[:, :], dr["x"][ti * 128:(ti + 1) * 128, :], xd)
                for half in range(2):
                    p, pd, _ = pSp.next()
                    hs = slice(half * 512, (half + 1) * 512)
                    kb.mm(p[:, :], pd,
                          [(mixa[:, g, sub * 128:(sub + 1) * 128], w_oa[:, g, hs]) for g in range(4)] +
                          [(ob[:, pr, sub * 128:(sub + 1) * 128], w_ob[:, pr, hs]) for pr in range(4)],
                          [mixad, w_oad, obd, w_obd])
                    kb.tt("dve", xt[:, hs], xt[:, hs], p[:, :], ALU.add, [xd, pd], [xd])
                kb.store("pool", xs, dr["x1_s"][ti * 128:(ti + 1) * 128, :], xt[:, :], xd, dr["_d_x1"][ti])
        if os.environ.get('KDEBUG'):
            print('sbuf remaining', nc.sbuf_bytes_remaining)
        S.barrier()
        S.release_dma_sems()


def phase_ffn(nc, S, dr, SEQ, l, src, src_dep, dst, dst_dep):
    with ExitStack() as st:
        kb = KB(nc, S, st)
        C = load_consts(kb, dr)
        TS = 512
        NST = SEQ // TS
        NF = DFF // 128
        stage = Pool(kb, "stg", 3, [128, 704], F32, dma=True)
        wg, wgd = kb.tile("wg", [128, 8, DFF], BF16)
        wu, wud = kb.tile("wu", [128, 8, DFF], BF16)
        wd, wdd = kb.tile("wd", [128, NF, D], BF16)
        gf, gfd, gs = kb.tile("gffn", [128, D], F32, dma=True)
        kb.load("sp", gs, gf[:, :], dr[f"gffn{l}"][:, :], gfd)
        load_cast_weight(kb, stage, wg, wgd, lambda c, c0, w: dr[f"wg{l}"][:, c, c0:c0 + w], 8, DFF, engs=("dve", "pool", "act"))
        load_cast_weight(kb, stage, wu, wud, lambda c, c0, w: dr[f"wu{l}"][:, c, c0:c0 + w], 8, DFF, engs=("dve", "pool", "act"))
        load_cast_weight(kb, stage, wd, wdd, lambda c, c0, w: dr[f"wd{l}"][:, c, c0:c0 + w], NF, D, engs=("dve", "pool", "act"))
        xin = Pool(kb, "xin", 6, [128, D], F32, dma=True)
        hnp = Pool(kb, "hn", 2, [128, D], BF16)
        pools = {"junk": hnp.tiles[0][:2], "ss": Pool(kb, "ss", 3, [128, 4], F32), "hn": hnp,
                 "psT": Pool(kb, "psT", 1, [128, 1024], BF16, space="psum")}
        pools["junk"] = None
        hTp = Pool(kb, "hT", 1, [128, 8, TS], BF16)
        pg = Pool(kb, "pg", 2, [128, 512], F32, space="psum")
        pu = Pool(kb, "pu", 2, [128, 512], F32, space="psum")
        pO = Pool(kb, "pO", 2, [128, 512], F32, space="psum")
        sgp = Pool(kb, "sg", 2, [128, TS], F32)
        hidp = Pool(kb, "hid", 1, [128, NF, TS], BF16)
        for stile in range(NST):
            t0 = stile * TS
            hT, hTd, _ = hTp.next()
            xts = []
            for sub in range(TS // 128):
                ti = t0 // 128 + sub
                xt, xd, xs = xin.next()
                kb.load("sp", xs, xt[:, :], src[ti * 128:(ti + 1) * 128, :], xd, reads=[src_dep[ti]])
                norm_transpose(kb, C, xt, xd, gf, gfd, hT, hTd, sub * 128, pools)
                xts.append((xt, xd, xs))
            hid, hidd, _ = hidp.next()
            for f in range(NF):
                a, ad, _ = pg.next()
                kb.mm(a[:, 0:TS], ad, [(wg[:, k, f * 128:(f + 1) * 128], hT[:, k, :]) for k in range(8)], [wgd, hTd])
                b, bd, _ = pu.next()
                kb.mm(b[:, 0:TS], bd, [(wu[:, k, f * 128:(f + 1) * 128], hT[:, k, :]) for k in range(8)], [wud, hTd])
                sg, sgd, _ = sgp.next()
                kb.act(sg[:, :], a[:, 0:TS], AF.Silu, [ad], [sgd])
                kb.tt("dve", hid[:, f, :], sg[:, :], b[:, 0:TS], ALU.mult, [sgd, bd], [hidd])
            for sub in range(TS // 128):
                ti = t0 // 128 + sub
                xt, xd, xs = xts[sub]
                for half in range(2):
                    hs = slice(half * 512, (half + 1) * 512)
                    p, pd, _ = pO.next()
                    kb.mm(p[:, :], pd, [(hid[:, f, sub * 128:(sub + 1) * 128], wd[:, f, hs]) for f in range(NF)],
                          [hidd, wdd])
                    kb.tt("dve", xt[:, hs], xt[:, hs], p[:, :], ALU.add, [xd, pd], [xd])
                kb.store("pool", xs, dst[ti * 128:(ti + 1) * 128, :], xt[:, :], xd, dst_dep[ti])
        if os.environ.get('KDEBUG'):
            print('sbuf remaining', nc.sbuf_bytes_remaining)
        S.barrier()
        S.release_dma_sems()


def phase_c1(nc, S, dr, SEQ):
    with ExitStack() as st:
        kb = KB(nc, S, st)
        C = load_consts(kb, dr)
        TS = 512
        NST = SEQ // TS
        stage = Pool(kb, "stg", 4, [128, 1024], F32, dma=True)
        w_in, w_ind = kb.tile("w_in1", [128, 8, OIN], BF16)
        gmix, gmixd, gs = kb.tile("gmix1", [128, D], F32, dma=True)
        kb.load("sp", gs, gmix[:, :], dr["gmix1"][:, :], gmixd)
        load_cast_weight(kb, stage, w_in, w_ind, lambda c, c0, w: dr["w_in1"][:, c, c0:c0 + w], 8, OIN,
                         engs=("dve", "pool", "act"))
        xin = Pool(kb, "xin", 3, [128, D], F32, dma=True)
        pools = {"junk": kb.tile("junk", [128, D], BF16), "ss": Pool(kb, "ss", 3, [128, 4], F32),
                 "hn": Pool(kb, "hn", 2, [128, D], BF16),
                 "psT": Pool(kb, "psT", 1, [128, 1024], BF16, space="psum")}
        hTp = Pool(kb, "hT", 1, [128, 8, TS], BF16)
        pA = Pool(kb, "pA", 2, [128, 512], F32, space="psum")
        pB = Pool(kb, "pB", 2, [128, 512], F32, space="psum")
        pv = Pool(kb, "pv", 3, [128, 512], F32, space="psum")
        cosp = Pool(kb, "cos", 2, [128, TS], F32, dma=True)
        sinp = Pool(kb, "sin", 2, [128, TS], F32, dma=True)
        tp = [Pool(kb, f"t{i}", 2, [128, TS], F32) for i in range(4)]
        qkp = Pool(kb, "qk", 2, [128, 4, 8, 128], BF16, dma=True)
        vtp = Pool(kb, "vt", 2, [128, 2048], BF16, dma=True)
        sgp = Pool(kb, "sgt", 2, [128, 2048], BF16, dma=True)
        for stile in range(NST):
            t0 = stile * TS
            hT, hTd, _ = hTp.next()
            for sub in range(4):
                ti = t0 // 128 + sub
                xt, xd, xs = xin.next()
                kb.load("sp", xs, xt[:, :], dr["x2_s"][ti * 128:(ti + 1) * 128, :], xd, reads=[dr["_d_x2"][ti]])
                norm_transpose(kb, C, xt, xd, gmix, gmixd, hT, hTd, sub * 128, pools)
            cs, csd, css = cosp.next()
            kb.load("sp", css, cs[:, :], dr["cosR"][:, t0:t0 + TS], csd)
            sn, snd, sns = sinp.next()
            kb.load("sp", sns, sn[:, :], dr["sinR"][:, t0:t0 + TS], snd)
            for which, col, dst, ddep in ((0, 0, "qT_s", "_d_qT"), (1, 1024, "kT_s", "_d_kT")):
                qk, qkd, qks = qkp.next()
                for h in range(4):
                    a, ad, _ = pA.next()
                    c0 = col + h * 256
                    kb.mm(a[:, :], ad, [(w_in[:, kk, c0:c0 + 128], hT[:, kk, :]) for kk in range(8)], [w_ind, hTd])
                    b, bd, _ = pB.next()
                    kb.mm(b[:, :], bd, [(w_in[:, kk, c0 + 128:c0 + 256], hT[:, kk, :]) for kk in range(8)], [w_ind, hTd])
                    t = [p.next() for p in tp]
                    kb.tt("dve", t[0][0][:, :], a[:, :], cs[:, :], ALU.mult, [ad, csd], [t[0][1]])
                    kb.tt("dve", t[1][0][:, :], b[:, :], sn[:, :], ALU.mult, [bd, snd], [t[1][1]])
                    kb.tt("pool", qk[:, :, h * 2, :], t[0][0][:, :].rearrange("p (s t) -> p s t", s=4),
                          t[1][0][:, :].rearrange("p (s t) -> p s t", s=4), ALU.subtract, [t[0][1], t[1][1]], [qkd])
                    kb.tt("dve", t[2][0][:, :], b[:, :], cs[:, :], ALU.mult, [bd, csd], [t[2][1]])
                    kb.tt("dve", t[3][0][:, :], a[:, :], sn[:, :], ALU.mult, [ad, snd], [t[3][1]])
                    kb.tt("pool", qk[:, :, h * 2 + 1, :], t[2][0][:, :].rearrange("p (s t) -> p s t", s=4),
                          t[3][0][:, :].rearrange("p (s t) -> p s t", s=4), ALU.add, [t[2][1], t[3][1]], [qkd])
                kb.store("pool", qks, dr[dst][stile * 4:(stile + 1) * 4, :, :, :].rearrange("s p c t -> p s c t"),
                         qk[:, :, :, :], qkd, dr[ddep][stile])
            for sub in range(4):
                ti = t0 // 128 + sub
                vt, vtd, vts = vtp.next()
                sg, sgd, sgs = sgp.next()
                for n in range(4):
                    p, pd, _ = pv.next()
                    kb.mm(p[:, :], pd, [(hT[:, kk, sub * 128:(sub + 1) * 128], w_in[:, kk, 2048 + n * 512:2048 + (n + 1) * 512])
                                        for kk in range(8)], [w_ind, hTd])
                    kb.cp("dve" if n % 2 == 0 else "act", vt[:, n * 512:(n + 1) * 512], p[:, :], [pd], [vtd])
                for n in range(4):
                    p, pd, _ = pv.next()
                    kb.mm(p[:, :], pd, [(hT[:, kk, sub * 128:(sub + 1) * 128], w_in[:, kk, 4096 + n * 512:4096 + (n + 1) * 512])
                                        for kk in range(8)], [w_ind, hTd])
                    kb.act(sg[:, n * 512:(n + 1) * 512], p[:, :], AF.Silu, [pd], [sgd])
                kb.store("pool", vts, dr["v_s"][ti * 128:(ti + 1) * 128, :], vt[:, :], vtd, dr["_d_v"][ti])
                kb.store("pool", sgs, dr["sg_s"][ti * 128:(ti + 1) * 128, :], sg[:, :], sgd, dr["_d_sg"][ti])
        if os.environ.get('KDEBUG'):
            print('sbuf remaining', nc.sbuf_bytes_remaining)
        S.barrier()
        S.release_dma_sems()


def phase_c2(nc, S, dr, SEQ, gchunk):
    with ExitStack() as st:
        kb = KB(nc, S, st)
        C = load_consts(kb, dr)
        eps, ident = C["eps"], C["ident"]
        NT = SEQ // 128
        stage = Pool(kb, "stg", 5, [128, 1024], F32, dma=True)
        w_o, w_od = kb.tile("w_out1", [128, 16, D], BF16)
        load_cast_weight(kb, stage, w_o, w_od, lambda c, c0, w: dr["w_out1"][:, c, c0:c0 + w], 16, D)
        gret, gretd, gs = kb.tile("gret", [128, 2048], F32, dma=True)
        kb.load("sp", gs, gret[:, :], dr["gret"][:, :], gretd)
        maskT, maskTd, gs = kb.tile("maskT", [128, 4, 128], F32, dma=True)
        kb.load("sp", gs, maskT[:, :, :], dr["maskT"][:, :, :], maskTd)
        zeta8, zeta8d, gs = kb.tile("zeta8", [128, 1024], F32, dma=True)
        kb.load("sp", gs, zeta8[:, :], dr["zeta8"][:, :], zeta8d)
        xi8, xi8d, gs = kb.tile("xi8", [128, 8, 128], F32, dma=True)
        kb.load("sp", gs, xi8[:, :, :], dr["xi8"][:, :, :], xi8d)
        Sf, _ = kb.tile("Sf", [128, 4, 2, 512], F32)
        Sb, _ = kb.tile("Sb", [128, 4, 2, 512], BF16)
        Sfd = [Dep(f"Sf{h}") for h in range(4)]
        Sbd = [Dep(f"Sb{h}") for h in range(4)]
        qp = Pool(kb, "q", 3, [128, 8, 128], BF16, dma=True)
        kp = Pool(kb, "k", 3, [128, 8, 128], BF16, dma=True)
        vp = Pool(kb, "v", 3, [128, 2048], BF16, dma=True)
        sgp = Pool(kb, "sg", 3, [128, 2048], BF16, dma=True)
        xin = Pool(kb, "xin", 4, [128, D], F32, dma=True)
        psT = Pool(kb, "psT", 1, [128, 1024], BF16, space="psum")
        paT = Pool(kb, "paT", 1, [128, 512], F32, space="psum")
        pop = Pool(kb, "po", 4, [128, 512], F32, space="psum")
        pstp = Pool(kb, "pst", 1, [128, 2, 512], F32, space="psum")
        pOp = paT
        kzp = Pool(kb, "kz", 2, [128, 1024], BF16)
        qxp = Pool(kb, "qx", 2, [128, 8, 128], BF16)
        aTp = Pool(kb, "aTs", 2, [128, 4, 128], BF16)
        ssp = Pool(kb, "ss", 6, [128, 4], F32)
        onp = Pool(kb, "on", 4, [128, 512], F32)
        junkp = Pool(kb, "junk", 4, [128, 512], BF16)
        gtp = Pool(kb, "gated", 2, [128, 2048], BF16)
        gTp = Pool(kb, "gT", 2, [128, 16, 128], BF16)
        def front(i):
            F = {}
            q, qd, qs = qp.next()
            kb.load("sp", qs, q[:, :, :], dr["qT_s"][i, :, :, :], qd, reads=[dr["_d_qT"][i // 4]])
            k, kd, ks = kp.next()
            kb.load("sp", ks, k[:, :, :], dr["kT_s"][i, :, :, :], kd, reads=[dr["_d_kT"][i // 4]])
            v, vd, vs = vp.next()
            kb.load("sp", vs, v[:, :], dr["v_s"][i * 128:(i + 1) * 128, :], vd, reads=[dr["_d_v"][i]])
            sg, sgd, sgs = sgp.next()
            kb.load("sp", sgs, sg[:, :], dr["sg_s"][i * 128:(i + 1) * 128, :], sgd, reads=[dr["_d_sg"][i]])
            xt, xd, xs = xin.next()
            kb.load("sp", xs, xt[:, :], dr["x2_s"][i * 128:(i + 1) * 128, :], xd, reads=[dr["_d_x2"][i]])
            aT, aTd, _ = paT.next()
            for h in range(4):
                kb.mm(aT[:, h * 128:(h + 1) * 128], aTd,
                      [(k[:, h * 2 + dc, :], q[:, h * 2 + dc, :]) for dc in range(2)], [kd, qd])
            aTs, aTsd, _ = aTp.next()
            kb.tt("dve", aTs[:, :, :], aT[:, :].rearrange("p (h j) -> p h j", h=4), maskT[:, :, :], ALU.mult,
                  [aTd, maskTd], [aTsd])
            qx, qxd = None, None
            if i > 0:
                qx, qxd, _ = qxp.next()
                kb.tt("pool", qx[:, :, :], q[:, :, :], xi8[:, :, :], ALU.mult, [qd, xi8d], [qxd])
            kz, kzd = None, None
            if i < NT - 1:
                pT, pTd, _ = psT.next()
                for c in range(8):
                    kb.tr(pT[:, c * 128:(c + 1) * 128], pTd, k[:, c, :], ident[0][:, :], [kd, ident[1]], inc=(c == 7))
                kz, kzd, _ = kzp.next()
                kb.tt("dve", kz[:, :], pT[:, :], zeta8[:, :], ALU.mult, [pTd, zeta8d], [kzd])
            F.update(q=q, qd=qd, k=k, kd=kd, v=v, vd=vd, sg=sg, sgd=sgd, xt=xt, xd=xd, xs=xs, aTs=aTs, aTsd=aTsd,
                     qx=qx, qxd=qxd, kz=kz, kzd=kzd)
            return F

        def middle(i, F):
            v, vd, sg, sgd = F["v"], F["vd"], F["sg"], F["sgd"]
            aTs, aTsd, qx, qxd, kz, kzd = F["aTs"], F["aTsd"], F["qx"], F["qxd"], F["kz"], F["kzd"]
            gated, gatedd, _ = gtp.next()
            H = []
            for h in range(4):
                vh = v[:, h * 512:(h + 1) * 512]
                po, pod, _ = pop.next()
                if i == 0:
                    kb.mm1(po[:, :], pod, aTs[:, h, :], vh, True, True, [aTsd, vd])
                else:
                    kb.mm1(po[:, :], pod, aTs[:, h, :], vh, True, False, [aTsd, vd], inc=False)
                    kb.mm1(po[:, :], pod, qx[:, h * 2, :], Sb[:, h, 0, :], False, False, [qxd, Sbd[h]], inc=False)
                    kb.mm1(po[:, :], pod, qx[:, h * 2 + 1, :], Sb[:, h, 1, :], False, True, [qxd, Sbd[h]])
                ss, ssd, _ = ssp.next()
                H.append((po, pod, ss, ssd))
            for h in range(4):
                po, pod, ss, ssd = H[h]
                junk, junkd, _ = junkp.next()
                kb.act(junk[:, :], po[:, :], AF.Square, [pod], [junkd, ssd], accum_out=ss[:, 0:1])
            if i < NT - 1:
                for h in range(4):
                    vh = v[:, h * 512:(h + 1) * 512]
                    pst, pstd, _ = pstp.next()
                    for dc in range(2):
                        kb.mm1(pst[:, dc, :], pstd, kz[:, h * 256 + dc * 128:h * 256 + (dc + 1) * 128], vh, True, True,
                               [kzd, vd], inc=(dc == 1))
                    if i == 0:
                        kb.cp("dve", Sf[:, h, :, :], pst[:, :, :], [pstd], [Sfd[h]])
                    else:
                        kb.stt("dve", Sf[:, h, :, :], Sf[:, h, :, :], float(gchunk[h]), pst[:, :, :], ALU.mult, ALU.add,
                               [Sfd[h], pstd], [Sfd[h]])
            for h in range(4):
                po, pod, ss, ssd = H[h]
                kb.act(ss[:, 0:1], ss[:, 0:1], AF.Ln, [ssd, eps[1]], [ssd], scale=1.0 / 512, bias=eps[0][:, 0:1])
            for h in range(4):
                po, pod, ss, ssd = H[h]
                kb.act(ss[:, 0:1], ss[:, 0:1], AF.Exp, [ssd], [ssd], scale=-0.5)
            if i < NT - 1:
                for h in range(4):
                    kb.cp("act", Sb[:, h, :, :], Sf[:, h, :, :], [Sfd[h]], [Sbd[h]])
            ons = []
            for h in range(4):
                po, pod, ss, ssd = H[h]
                on, ond, _ = onp.next()
                kb.stt("dve", on[:, :], po[:, :], ss[:, 0:1], gret[:, h * 512:(h + 1) * 512], ALU.mult, ALU.mult,
                       [pod, ssd, gretd], [ond])
                ons.append((on, ond))
            for h in range(4):
                on, ond = ons[h]
                kb.tt("pool", gated[:, h * 512:(h + 1) * 512], on[:, :], sg[:, h * 512:(h + 1) * 512], ALU.mult,
                      [ond, sgd], [gatedd])
            F["gated"], F["gatedd"] = gated, gatedd

        def back(i, F):
            gated, gatedd, xt, xd, xs = F["gated"], F["gatedd"], F["xt"], F["xd"], F["xs"]
            gT, gTd, _ = gTp.next()
            for half in range(2):
                pT, pTd, _ = psT.next()
                for c in range(8):
                    cc = half * 8 + c
                    kb.tr(pT[:, c * 128:(c + 1) * 128], pTd, gated[:, cc * 128:(cc + 1) * 128], ident[0][:, :],
                          [gatedd, ident[1]], inc=(c == 7))
                kb.cp("act", gT[:, half * 8:(half + 1) * 8, :], pT[:, :].rearrange("p (c t) -> p c t", c=8), [pTd], [gTd])
            for half in range(2):
                hs = slice(half * 512, (half + 1) * 512)
                p, pd, _ = pOp.next()
                kb.mm(p[:, :], pd, [(gT[:, c, :], w_o[:, c, hs]) for c in range(16)], [gTd, w_od])
                kb.tt("dve", xt[:, hs], xt[:, hs], p[:, :], ALU.add, [xd, pd], [xd])
            kb.store("pool", xs, dr["x3_s"][i * 128:(i + 1) * 128, :], xt[:, :], xd, dr["_d_x3"][i])

        Fs = {0: front(0)}
        middle(0, Fs[0])
        if NT > 1:
            Fs[1] = front(1)
        for i in range(NT):
            if i + 1 < NT:
                middle(i + 1, Fs[i + 1])
            if i + 2 < NT:
                Fs[i + 2] = front(i + 2)
            back(i, Fs.pop(i))
        if os.environ.get('KDEBUG'):
            print('sbuf remaining', nc.sbuf_bytes_remaining)
        S.barrier()
        S.release_dma_sems()


class LazyDR(dict):
    def __init__(self, nc, specs):
        super().__init__()
        self.nc = nc
        self.specs = specs
        self.used = []

    def __missing__(self, name):
        shape, dt = self.specs[name]
        ap = self.nc.dram_tensor(name, list(shape), dt, kind="ExternalInput").ap()
        self[name] = ap
        self.used.append(name)
        return ap


def build_program(SEQ, debug=None, upto="all"):
    nc = bass.Bass("TRN2", target_bir_lowering=False)
    specs = {"x": ([SEQ, D], F32)}
    for nm, shp, dt in (("ident", [128, 128], BF16), ("ones", [128, 128], BF16), ("eps", [128, 1], F32),
                        ("ident32x2", [128, 64], F32), ("gmix0", [128, D], F32), ("gvn", [128, 512], F32), ("bsb", [128, 512], F32),
                        ("gqa", [128, 3], F32), ("gkva", [128, 2], F32),
                        ("w_in0", [128, 8, EIN], F32), ("wsT", [128, 4, 128], F32),
                        ("w_uq", [128, 3, 768], F32), ("w_ukv", [128, 2, 1024], F32),
                        ("w_oa", [128, 4, D], F32), ("w_ob", [128, 4, D], F32),
                        ("gq", [96, 1], F32), ("gk", [96, 1], F32), ("pfull", [96, 96], BF16),
                        ("cosM", [32, SEQ], F32), ("sinM", [32, SEQ], F32),
                        ("gffn0", [128, D], F32), ("wg0", [128, 8, DFF], F32), ("wu0", [128, 8, DFF], F32),
                        ("wd0", [128, DFF // 128, D], F32),
                        ("gffn1", [128, D], F32), ("wg1", [128, 8, DFF], F32), ("wu1", [128, 8, DFF], F32),
                        ("wd1", [128, DFF // 128, D], F32),
                        ("gmix1", [128, D], F32), ("w_in1", [128, 8, OIN], F32), ("w_out1", [128, 16, D], F32),
                        ("cosR", [128, SEQ], F32), ("sinR", [128, SEQ], F32), ("gret", [128, 2048], F32),
                        ("maskT", [128, 4, 128], F32), ("xi8", [128, 8, 128], F32), ("zeta8", [128, 1024], F32),
                        ):
        specs[nm] = (shp, dt)
    dr = LazyDR(nc, specs)
    kind = "ExternalOutput" if debug else "Internal"
    NST = SEQ // 512
    NT = SEQ // 128

    def scratch(name, shape, dt, ntiles):
        dr[name] = nc.dram_tensor(name, list(shape), dt, kind=kind).ap()
        dr["_d_" + name[:-2]] = [Dep(name + str(i)) for i in range(ntiles)]
    scratch("mixa_s", [4, 128, SEQ], BF16, NST)
    scratch("cqn_s", [3, 128, SEQ], BF16, NST)
    scratch("ckvn_s", [2, 128, SEQ], BF16, NST)
    scratch("kpe_s", [32, SEQ], F32, NST)
    scratch("x1_s", [SEQ, D], F32, NT)
    scratch("x2_s", [SEQ, D], F32, NT)
    scratch("x3_s", [SEQ, D], F32, NT)
    scratch("qT_s", [NT, 128, 8, 128], BF16, NST)
    scratch("kT_s", [NT, 128, 8, 128], BF16, NST)
    scratch("v_s", [SEQ, 2048], BF16, NT)
    scratch("sg_s", [SEQ, 2048], BF16, NT)
    dr["out"] = nc.dram_tensor("out", [SEQ, D], F32, kind="ExternalOutput").ap()
    out_dep = [Dep(f"out{i}") for i in range(NT)]
    order = ["a1", "a2", "b", "c1", "c2", "d"]
    last = order.index(upto) if upto in order else len(order) - 1
    with ExitStack() as st:
        S = Sched(nc, st)
        phase_a1(nc, S, dr, SEQ)
        if last >= 1:
            phase_a2(nc, S, dr, SEQ)
        if last >= 2:
            phase_ffn(nc, S, dr, SEQ, 0, dr["x1_s"], dr["_d_x1"], dr["x2_s"], dr["_d_x2"])
        if last >= 3:
            phase_c1(nc, S, dr, SEQ)
        if last >= 4:
            phase_c2(nc, S, dr, SEQ, ret_consts()[3])
        if last >= 5:
            phase_ffn(nc, S, dr, SEQ, 1, dr["x3_s"], dr["_d_x3"], dr["out"], out_dep)
        S.barrier()
        print("ops", S.n_ops, "waits", S.n_wait, "sems", S.nsem)
    nc._used_inputs = list(dr.used)
    return nc


def host_inputs(inp, b, SEQ):
    f = np.float32
    c = np.ascontiguousarray
    m = {}
    m["x"] = c(inp["x"][b])
    m["ident"] = np.eye(128, dtype=ml_dtypes.bfloat16)
    m["ones"] = np.ones((128, 128), dtype=ml_dtypes.bfloat16)
    m["ident32x2"] = np.concatenate([np.eye(64, dtype=f), np.eye(64, dtype=f)], 0)
    m["eps"] = np.full((128, 1), EPS, dtype=f)
    m["gmix0"] = c(np.broadcast_to(inp["norm_mix"][0][None, :], (128, D)))
    m["gvn"] = c(np.broadcast_to(inp["gm_v_norm"][0].reshape(1, 512), (128, 512)))
    m["bsb"] = c(np.broadcast_to(inp["gm_b_s"][0].reshape(1, 512), (128, 512)))
    m["gqa"] = c(inp["mla_q_a_norm"][0].reshape(3, 128).T)
    m["gkva"] = c(inp["mla_kv_a_norm"][0].reshape(2, 128).T)
    m["w_in0"] = c(inp["even_w_in"][0].reshape(8, 128, EIN).transpose(1, 0, 2))
    m["wsT"] = c(inp["gm_w_s"][0].transpose(2, 0, 1))
    m["w_uq"] = c(inp["mla_w_uq"][0].reshape(3, 128, 768).transpose(1, 0, 2))
    m["w_ukv"] = c(inp["mla_w_ukv"][0].reshape(2, 128, 1024).transpose(1, 0, 2))
    wo = inp["even_w_out"][0]
    m["w_oa"] = c(wo[0:512].reshape(4, 128, D).transpose(1, 0, 2))
    m["w_ob"] = c(wo[512:1024].reshape(4, 128, D).transpose(1, 0, 2))
    m["gq"] = c(inp["mla_q_norm"][0].reshape(96, 1))
    m["gk"] = c(inp["mla_k_norm"][0].reshape(96, 1))
    m.update(const_tables(SEQ))
    m["gmix1"] = c(np.broadcast_to(inp["norm_mix"][1][None, :], (128, D)))
    m["w_in1"] = c(inp["odd_w_in"][0].reshape(8, 128, OIN).transpose(1, 0, 2))
    m["w_out1"] = c(inp["odd_w_out"][0].reshape(16, 128, D).transpose(1, 0, 2))
    m["gret"] = c(np.broadcast_to(inp["ret_out_norm"][0].reshape(1, 2048), (128, 2048)))
    for l in range(2):
        m[f"gffn{l}"] = c(np.broadcast_to(inp["norm_ffn"][l][None, :], (128, D)))
        m[f"wg{l}"] = c(inp["ffn_w_gate"][l].reshape(8, 128, DFF).transpose(1, 0, 2))
        m[f"wu{l}"] = c(inp["ffn_w_up"][l].reshape(8, 128, DFF).transpose(1, 0, 2))
        m[f"wd{l}"] = c(inp["ffn_w_down"][l].reshape(DFF // 128, 128, D).transpose(1, 0, 2))
    return m


_CT = {}


def ret_consts():
    H, CH = 4, 128
    lg = np.log(1.0 - 2.0 ** (-5.0 - np.arange(H, dtype=np.float64)))
    j = np.arange(CH, dtype=np.float64)
    diff = j[None, :] - j[:, None]
    sc = 256.0 ** -0.5
    maskT = np.zeros((CH, H, CH), dtype=np.float64)
    for h in range(H):
        maskT[:, h, :] = np.where(diff >= 0, np.exp(lg[h] * np.maximum(diff, 0.0)), 0.0) * sc
    xi = np.exp(lg[:, None] * (j[None, :] + 1.0))
    xib = np.broadcast_to(xi[None, :, :], (128, H, CH))
    zeta = (np.exp(lg[None, :] * (CH - 1.0 - j[:, None])) * sc)
    gchunk = np.exp(lg * CH)
    return (np.ascontiguousarray(maskT.astype(np.float32)), np.ascontiguousarray(xib.astype(np.float32)),
            np.ascontiguousarray(zeta.astype(np.float32)), gchunk)


def const_tables(SEQ):
    if SEQ in _CT:
        return _CT[SEQ]
    f = np.float32
    m = {}
    pos = np.arange(SEQ, dtype=f)
    inv = (np.float32(10000.0) ** (-np.arange(16, dtype=f) / np.float32(16))).astype(f)
    ang = (pos[None, :] * inv[:, None]).astype(f)
    m["cosM"] = np.ascontiguousarray(np.concatenate([np.cos(ang), np.cos(ang)], 0).astype(f))
    m["sinM"] = np.ascontiguousarray(np.concatenate([np.sin(ang), np.sin(ang)], 0).astype(f))
    pfull = np.zeros((96, 96), dtype=f)
    for i in range(16):
        pfull[64 + i + 16, 64 + i] = -1.0
        pfull[64 + i, 64 + i + 16] = 1.0
    m["pfull"] = pfull.astype(ml_dtypes.bfloat16)
    invr = (np.float32(10000.0) ** (-np.arange(128, dtype=f) / np.float32(128))).astype(f)
    angr = (pos[None, :] * invr[:, None]).astype(f)
    m["cosR"] = np.ascontiguousarray(np.cos(angr).astype(f))
    m["sinR"] = np.ascontiguousarray(np.sin(angr).astype(f))
    maskT, xib, zeta, _ = ret_consts()
    m["maskT"] = maskT
    m["xi8"] = np.ascontiguousarray(np.repeat(xib, 2, axis=1))
    m["zeta8"] = np.ascontiguousarray(np.repeat(zeta, 256, axis=1))
    _CT[SEQ] = m
    return m


def kernel(**inputs):
    inp = {k: np.asarray(v) for k, v in inputs.items()}
    B, SEQ, _ = inp["x"].shape
    nc = build_program(SEQ)
    in_maps = [{k: v for k, v in host_inputs(inp, b, SEQ).items() if k in nc._used_inputs} for b in range(B)]
    res = run_bass_kernel_spmd(nc, in_maps, core_ids=list(range(B)))
    return np.stack([np.asarray(r["out"]) for r in res.results], axis=0).astype(np.float32)
```
